# Optimizing a Trainium2 kernel written in Bass

```python
import jax, jax.numpy as jnp
from jax import lax
import numpy as np

D_MODEL = 1024
BATCH = 8
SEQ = 4096
DEPTH = 1

HEAD_DIM = 64
RET_HEADS = 8
SWA_Q_HEADS = 8
SWA_KV_HEADS = 2
SWA_GROUP = SWA_Q_HEADS // SWA_KV_HEADS
RET_WIDTH = RET_HEADS * HEAD_DIM
SWA_WIDTH = SWA_Q_HEADS * HEAD_DIM
KV_WIDTH = SWA_KV_HEADS * HEAD_DIM
MIX_WIDTH = RET_WIDTH + SWA_WIDTH
IN_WIDTH = 4 * RET_WIDTH + SWA_WIDTH + 2 * KV_WIDTH
CHUNK = 128
WINDOW = 128
RET_THETA = 10000.0
SWA_THETA = 500000.0
SWA_ROT_DIM = HEAD_DIM // 4
N_EXPERTS = 64
TOP_K = 8
N_GROUPS = 8
TOPK_GROUPS = 4
EXPERT_DIM = 256
SHARED_DIM = 256
ROUTED_SCALE = 2.5
PLE_DIM = 256
LN_EPS = 1e-5
GN_EPS = 1e-6
NEG_INF = -1e30
DEEPNORM_ALPHA = (2.0 * DEPTH) ** 0.25
DEEPNORM_BETA = (8.0 * DEPTH) ** -0.25

kernel_name = "hymba_retnet_swa_sink_moe_deepnorm"


def rope(x, rot_dim, theta):
    s = x.shape[1]
    half = rot_dim // 2
    inv_freq = theta ** (-jnp.arange(half, dtype=jnp.float32) / half)
    ang = jnp.arange(s, dtype=jnp.float32)[:, None] * inv_freq[None, :]
    cos = jnp.cos(ang)[None, :, None, :].astype(x.dtype)
    sin = jnp.sin(ang)[None, :, None, :].astype(x.dtype)
    x1, x2, rest = x[..., :half], x[..., half:rot_dim], x[..., rot_dim:]
    return jnp.concatenate([x1 * cos - x2 * sin, x2 * cos + x1 * sin, rest], axis=-1)


def layer_norm(x, g, b):
    xf = x.astype(jnp.float32)
    mu = jnp.mean(xf, axis=-1, keepdims=True)
    var = jnp.mean(jnp.square(xf - mu), axis=-1, keepdims=True)
    y = (xf - mu) * lax.rsqrt(var + LN_EPS)
    return (y * g.astype(jnp.float32) + b.astype(jnp.float32)).astype(x.dtype)


def head_group_norm(y, gain):
    yf = y.astype(jnp.float32)
    mu = jnp.mean(yf, axis=-1, keepdims=True)
    var = jnp.mean(jnp.square(yf - mu), axis=-1, keepdims=True)
    yn = ((yf - mu) * lax.rsqrt(var + GN_EPS)).astype(y.dtype)
    b, s, h, d = y.shape
    return yn.reshape(b, s, h * d) * gain


def retention_chunkwise(q, k, v):
    b, s, h, d = q.shape
    n = s // CHUNK
    k = k * (d ** -0.5)
    q, k, v = [t.reshape(b, n, CHUNK, h, d) for t in (q, k, v)]
    log_g = jnp.log1p(-(2.0 ** (-5.0 - jnp.arange(h, dtype=jnp.float32))))
    idx = jnp.arange(CHUNK, dtype=jnp.float32)
    diff = idx[:, None] - idx[None, :]
    intra_decay = jnp.where(diff[None] >= 0,
                            jnp.exp(jnp.maximum(diff, 0.0)[None] * log_g[:, None, None]), 0.0)
    scores = jnp.einsum('bnqhd,bnkhd->bnhqk', q, k) * intra_decay.astype(q.dtype)
    intra = jnp.einsum('bnhqk,bnkhd->bnqhd', scores, v)
    k_w = jnp.exp((CHUNK - 1 - idx)[:, None] * log_g[None, :]).astype(k.dtype)
    q_w = jnp.exp((idx + 1)[:, None] * log_g[None, :]).astype(q.dtype)
    chunk_decay = jnp.exp(CHUNK * log_g)
    kv = jnp.einsum('bnkhd,bnkhe->nbhde', k * k_w[:, :, None], v)

    def step(state, kv_n):
        return state * chunk_decay[None, :, None, None].astype(state.dtype) + kv_n, state

    _, states = lax.scan(step, jnp.zeros_like(kv[0]), kv)
    inter = jnp.einsum('bnqhd,nbhde->bnqhe', q * q_w[:, :, None], states)
    return (intra + inter).reshape(b, s, h, d)


def swa_with_sinks(q, k, v, sinks):
    b, s, hq, d = q.shape
    n = s // WINDOW
    c = WINDOW
    q = q.reshape(b, n, c, SWA_KV_HEADS, SWA_GROUP, d)

    def band(t):
        t = t.reshape(b, n, c, SWA_KV_HEADS, d)
        prev = jnp.concatenate([jnp.zeros_like(t[:, :1]), t[:, :-1]], axis=1)
        return jnp.concatenate([prev, t], axis=2)

    kb, vb = band(k), band(v)
    scores = jnp.einsum('bnqhgd,bnkhd->bhgnqk', q, kb).astype(jnp.float32) * (d ** -0.5)
    qi = jnp.arange(c)[:, None]
    kj = jnp.arange(2 * c)[None, :]
    rel = c + qi - kj
    blk = jnp.arange(n)[:, None, None]
    valid = (rel >= 0) & (rel < WINDOW) & (blk * c + kj - c >= 0)
    scores = jnp.where(valid, scores, NEG_INF)
    sink = jnp.broadcast_to(sinks.astype(jnp.float32).reshape(1, SWA_KV_HEADS, SWA_GROUP, 1, 1, 1),
                            scores.shape[:-1] + (1,))
    probs = jax.nn.softmax(jnp.concatenate([scores, sink], axis=-1), axis=-1)[..., :-1]
    out = jnp.einsum('bhgnqk,bnkhd->bnqhgd', probs.astype(v.dtype), vb)
    return out.reshape(b, s, hq * d)


def hybrid_mixer(x, w_in, ret_gn_gain, attn_scale, sinks, w_out):
    b, s, _ = x.shape
    proj = x @ w_in
    splits = [RET_WIDTH, 2 * RET_WIDTH, 3 * RET_WIDTH, 4 * RET_WIDTH,
              4 * RET_WIDTH + SWA_WIDTH, 4 * RET_WIDTH + SWA_WIDTH + KV_WIDTH]
    rq, rk, rv, rg, aq, ak, av = jnp.split(proj, splits, axis=-1)
    rq = rope(rq.reshape(b, s, RET_HEADS, HEAD_DIM), HEAD_DIM, RET_THETA)
    rk = rope(rk.reshape(b, s, RET_HEADS, HEAD_DIM), HEAD_DIM, RET_THETA)
    rv = rv.reshape(b, s, RET_HEADS, HEAD_DIM)
    ret = retention_chunkwise(rq, rk, rv)
    ret = jax.nn.silu(rg) * head_group_norm(ret, ret_gn_gain)
    aq = rope(aq.reshape(b, s, SWA_Q_HEADS, HEAD_DIM), SWA_ROT_DIM, SWA_THETA)
    ak = rope(ak.reshape(b, s, SWA_KV_HEADS, HEAD_DIM), SWA_ROT_DIM, SWA_THETA)
    av = av.reshape(b, s, SWA_KV_HEADS, HEAD_DIM)
    att = swa_with_sinks(aq, ak, av, sinks) * attn_scale
    return jnp.concatenate([ret, att], axis=-1) @ w_out


def moe_ffn(x, w_router, router_bias, w1, w3, w2, ws1, ws3, ws2):
    b, s, d = x.shape
    t = x.reshape(b * s, d)
    scores = jax.nn.sigmoid((t @ w_router).astype(jnp.float32))
    biased = scores + router_bias.astype(jnp.float32)
    grp = biased.reshape(-1, N_GROUPS, N_EXPERTS // N_GROUPS)
    grp_score = jnp.sum(lax.top_k(grp, 2)[0], axis=-1)
    _, top_grp = lax.top_k(grp_score, TOPK_GROUPS)
    grp_mask = jnp.sum(jax.nn.one_hot(top_grp, N_GROUPS, dtype=jnp.float32), axis=1) > 0
    expert_mask = jnp.repeat(grp_mask, N_EXPERTS // N_GROUPS, axis=1)
    _, idx = lax.top_k(jnp.where(expert_mask, biased, NEG_INF), TOP_K)
    w = jnp.take_along_axis(scores, idx, axis=1)
    w = w / jnp.sum(w, axis=-1, keepdims=True) * ROUTED_SCALE
    combine = jnp.sum(jax.nn.one_hot(idx, N_EXPERTS, dtype=jnp.float32) * w[..., None],
                      axis=1).astype(x.dtype)

    def expert_step(acc, params):
        e1, e3, e2, c = params
        h = jax.nn.silu(t @ e1) * (t @ e3)
        return acc + c[:, None] * (h @ e2), None

    routed, _ = lax.scan(expert_step, jnp.zeros_like(t), (w1, w3, w2, combine.T))
    shared = (jax.nn.silu(t @ ws1) * (t @ ws3)) @ ws2
    return (routed + shared).reshape(b, s, d)


def setup_inputs(seed: int = 0) -> dict:
    key = jax.random.key(seed)
    ks = jax.random.split(key, 24)
    L, D, E, F, SF = DEPTH, D_MODEL, N_EXPERTS, EXPERT_DIM, SHARED_DIM
    nrm = lambda k, shape, scale: jax.random.normal(k, shape, jnp.float32) * scale
    return {
        "x": nrm(ks[0], (BATCH, SEQ, D), 1.0),
        "p": nrm(ks[1], (L, BATCH, SEQ, PLE_DIM), 1.0),
        "w_in": nrm(ks[2], (L, D, IN_WIDTH), D ** -0.5),
        "ret_gn_gain": 1.0 + nrm(ks[3], (L, RET_WIDTH), 0.02),
        "attn_scale": 1.0 + nrm(ks[4], (L, SWA_WIDTH), 0.02),
        "sinks": nrm(ks[5], (L, SWA_Q_HEADS), 0.5),
        "w_out": nrm(ks[6], (L, MIX_WIDTH, D), MIX_WIDTH ** -0.5 * DEEPNORM_BETA),
        "ln1_g": 1.0 + nrm(ks[7], (L, D), 0.02),
        "ln1_b": nrm(ks[8], (L, D), 0.02),
        "w_router": nrm(ks[9], (L, D, E), D ** -0.5),
        "router_bias": nrm(ks[10], (L, E), 0.01),
        "w1": nrm(ks[11], (L, E, D, F), D ** -0.5),
        "w3": nrm(ks[12], (L, E, D, F), D ** -0.5),
        "w2": nrm(ks[13], (L, E, F, D), F ** -0.5 * DEEPNORM_BETA),
        "ws1": nrm(ks[14], (L, D, SF), D ** -0.5),
        "ws3": nrm(ks[15], (L, D, SF), D ** -0.5),
        "ws2": nrm(ks[16], (L, SF, D), SF ** -0.5 * DEEPNORM_BETA),
        "w_ple_gate": nrm(ks[17], (L, D, D), D ** -0.5),
        "b_ple_gate": nrm(ks[18], (L, D), 0.02),
        "w_ple_proj": nrm(ks[19], (L, PLE_DIM, D), PLE_DIM ** -0.5 * DEEPNORM_BETA),
        "ln2_g": 1.0 + nrm(ks[20], (L, D), 0.02),
        "ln2_b": nrm(ks[21], (L, D), 0.02),
    }


def reference(x, p, w_in, ret_gn_gain, attn_scale, sinks, w_out, ln1_g, ln1_b,
              w_router, router_bias, w1, w3, w2, ws1, ws3, ws2,
              w_ple_gate, b_ple_gate, w_ple_proj, ln2_g, ln2_b):
    h = x
    for i in range(DEPTH):
        mix = hybrid_mixer(h, w_in[i], ret_gn_gain[i], attn_scale[i], sinks[i], w_out[i])
        h = layer_norm(DEEPNORM_ALPHA * h + mix, ln1_g[i], ln1_b[i])
        ffn = moe_ffn(h, w_router[i], router_bias[i], w1[i], w3[i], w2[i], ws1[i], ws3[i], ws2[i])
        ple = jax.nn.sigmoid(h @ w_ple_gate[i] + b_ple_gate[i]) * (p[i] @ w_ple_proj[i])
        h = layer_norm(DEEPNORM_ALPHA * h + ffn + ple, ln2_g[i], ln2_b[i])
    return h
```

```python
import os
import numpy as np
from contextlib import ExitStack
import concourse.bass as bass
import concourse.mybir as mybir
from concourse.bass_utils import run_bass_kernel_spmd

F32 = mybir.dt.float32
BF16 = mybir.dt.bfloat16
I32 = mybir.dt.int32
AF = mybir.ActivationFunctionType
ALU = mybir.AluOpType
AX = mybir.AxisListType

S_LEN = 4096
D = 1024
NCH = S_LEN // 128
NE = 64
CAP = 768
NJ = CAP // 128
ALPHA = 2.0 ** 0.25
ROPE_ADD_ENG = "dve"
SKIP_AK_ROPE = False
DUMMY_TOK = S_LEN
DUMMY_SLOT = NE * CAP


class Sync:
    def __init__(self, nc, stack):
        self.nc = nc
        self.stack = stack
        self.eng = {"pe": nc.tensor, "act": nc.scalar, "dve": nc.vector, "pool": nc.gpsimd, "sp": nc.sync}
        self.sems = {}
        self.cnt = {}
        for e in ("pe", "act", "dve", "pool"):
            self.sems[e] = stack.enter_context(nc.semaphore("s_" + e))
            self.cnt[e] = 0
        self.waited = {e: {} for e in self.eng}
        self.last_w = {}
        self.readers = {}
        self.q = {e: [] for e in self.eng}
        self.sp_recent = []
        self.excl = set(["TB0", "TB1a", "TB1b", "F0", "F1", "F2", "F3", "F4", "TP0", "TP1", "HB0", "HB1", "HB2", "HB3", "YB0", "YB1"])

    def dsem(self, name):
        s = self.stack.enter_context(self.nc.semaphore("d_" + name))
        self.sems["d_" + name] = s
        self.cnt["d_" + name] = 0
        return "d_" + name

    def _need(self, e, key, val):
        if self.waited[e].get(key, 0) >= val:
            return
        self.waited[e][key] = val
        self.q[e].append(("wait", self.sems[key], val))

    def _deps(self, e, reads, writes, skip=None):
        for r in reads:
            w = self.last_w.get(r)
            if w is not None and not (w[0] == e == "pe"):
                self._need(e, w[0], w[1])
            if r in self.excl:
                for (k, v) in self.readers.get(r, []):
                    if k != e:
                        self._need(e, k, v)
        for wkey in writes:
            w = self.last_w.get(wkey)
            if w is not None and not (w[0] == e == "pe") and w[0] != skip:
                self._need(e, w[0], w[1])
            for (k, v) in self.readers.get(wkey, []):
                if not (k == e == "pe") and k != skip:
                    self._need(e, k, v)

    def barrier(self, dsems=()):
        for e in ("pe", "act", "dve", "pool", "sp"):
            for k in ("pe", "act", "dve", "pool"):
                if k != e and self.cnt[k] > 0:
                    self._need(e, k, self.cnt[k])
            for k in dsems:
                if self.cnt[k] > 0:
                    self._need(e, k, self.cnt[k])

    def _commit(self, key, val, reads, writes):
        for r in reads:
            self.readers.setdefault(r, []).append((key, val))
        for w in writes:
            self.last_w[w] = (key, val)
            self.readers[w] = []

    def op(self, e, fn, reads=(), writes=(), inc=True):
        self._deps(e, reads, writes)
        val = self.cnt[e] + 1
        self._commit(e, val, reads, writes)
        self.q[e].append(("ins", fn, self.sems[e] if inc else None, 1))
        if inc:
            self.cnt[e] = val

    def dma(self, q, out, in_, dsem, reads=(), writes=(), **kw):
        self._deps(q, reads, writes, skip=dsem)
        self.q[q].append(("ins", L("dma_start", out=out, in_=in_, **kw), self.sems[dsem], 16))
        self.cnt[dsem] += 16
        self._commit(dsem, self.cnt[dsem], reads, writes)

    def idma(self, out, out_off, in_, in_off, dsem, reads=(), writes=(), **kw):
        self._deps("pool", reads, writes, skip=dsem)
        self.q["pool"].append(("ins", L("indirect_dma_start", out=out, out_offset=out_off, in_=in_, in_offset=in_off, **kw), self.sems[dsem], 16))
        self.cnt[dsem] += 16
        self._commit(dsem, self.cnt[dsem], reads, writes)

    def flush(self):
        def replay(items):
            def run(eng):
                for it in items:
                    if it[0] == "wait":
                        eng.wait_ge(it[1], it[2])
                    else:
                        ins = it[1](eng)
                        if it[2] is not None:
                            ins.then_inc(it[2], it[3])
            return run
        qs = self.q
        self.q = {e: [] for e in self.eng}
        with self.nc.Block() as block:
            block.tensor(replay(qs["pe"]))
            block.scalar(replay(qs["act"]))
            block.vector(replay(qs["dve"]))
            block.gpsimd(replay(qs["pool"]))
            block.sync(replay(qs["sp"]))

    def wait_all(self, e, keys):
        for k in keys:
            w = self.last_w.get(k)
            if w is not None:
                self._need(e, w[0], w[1])


def L(name, *a, **k):
    return lambda e: getattr(e, name)(*a, **k)


def bc(ap, shape, axis):
    return ap.unsqueeze(axis).to_broadcast(list(shape))


def build(debug=False, upto=3, nrun=NCH, stage=99):
    nc = bass.Bass("TRN2", target_bir_lowering=False)

    def din(name, shape, dt=F32):
        return nc.dram_tensor(name, list(shape), dt, kind="ExternalInput").ap()

    x = din("x", [S_LEN, D]); p_in = din("p", [S_LEN, 256])
    w_in = din("w_in", [D, 2816]); w_out = din("w_out", [D, D]); w_router = din("w_router", [D, NE])
    NEd = 1 if upto == 1 else NE
    w1 = din("w1", [NEd, D, 256]); w3 = din("w3", [NEd, D, 256]); w2 = din("w2", [NEd, 256, D])
    ws1 = din("ws1", [D, 256]); ws3 = din("ws3", [D, 256]); ws2 = din("ws2", [256, D])
    w_pg = din("w_ple_gate", [D, D]); w_pp = din("w_ple_proj", [256, D])
    gain_r = din("gain_r", [128, 512]); ascale_r = din("ascale_r", [128, 512]); sinks_r = din("sinks_r", [128, 8])
    ln1g_r = din("ln1g_r", [128, D]); ln1b_r = din("ln1b_r", [128, D]); bgate_r = din("bgate_r", [128, D])
    ln2g_r = din("ln2g_r", [128, D]); ln2b_r = din("ln2b_r", [128, D]); rbias_r = din("rbias_r", [128, NE])
    c_ident = din("c_ident", [128, 128]); c_rope = din("c_rope", [NCH, 128, 160]); c_dt = din("c_dt", [128, 1024])
    c_qw = din("c_qw", [128, 512]); c_kw = din("c_kw", [128, 8]); c_cd = din("c_cd", [128, 256])
    c_mcur = din("c_mcur", [128, 128]); c_mprev = din("c_mprev", [128, 128]); c_utri = din("c_utri", [128, 128])
    c_ones = din("c_ones", [128, 128]); c_eoff = din("c_eoff", [128, NE]); c_tok = din("c_tok", [128, NCH], I32)
    okind = "ExternalOutput" if debug else "Internal"
    out = nc.dram_tensor("out", [S_LEN, D], F32, kind="ExternalOutput").ap()
    x1d = nc.dram_tensor("x1d", [S_LEN + 1, D], BF16, kind=okind).ap()
    based = nc.dram_tensor("based", [S_LEN, D], F32, kind=okind).ap()
    cmatd = nc.dram_tensor("cmatd", [S_LEN + 1, NE], F32, kind=okind).ap()
    tl = nc.dram_tensor("tl", [NE * CAP + 128, 1], I32, kind=okind).ap()
    Yd = nc.dram_tensor("Yd", [NE * CAP + 1, D], F32, kind=okind).ap()

    with ExitStack() as st0:
        S = Sync(nc, st0)
        sb0 = lambda n, s, d=F32: st0.enter_context(nc.sbuf_tensor(n, list(s), d))
        slots = sb0("slots", [128, NCH, 8], I32)
        ln2g = sb0("ln2g", [128, D]); ln2b = sb0("ln2b", [128, D])
        identb = sb0("identb", [128, 128], BF16)
        d_misc = S.dsem("misc")
        d_init = S.dsem("init")
        S.dma("sp", ln2g[:], ln2g_r, d_misc, writes=["ln2g"])
        S.dma("sp", ln2b[:], ln2b_r, d_misc, writes=["ln2b"])
        d_miscp = S.dsem("miscp")
        S.dma("pool", identb[:], c_ident, d_miscp, writes=["identb"])

        with ExitStack() as st:
            sb = lambda n, s, d=F32: st.enter_context(nc.sbuf_tensor(n, list(s), d))
            ps = lambda n, s, d=F32: st.enter_context(nc.psum_tensor(n, list(s), d))
            winb = sb("winb", [128, 8, 2816], BF16); woutb = sb("woutb", [128, 8, D], BF16)
            wr = sb("wr", [128, 8, NE]); wsb = sb("wsb", [128, 8, 512], BF16); ws2b = sb("ws2b", [128, 2, D], BF16)
            wgb = sb("wgb", [128, 8, D], BF16); wpb = sb("wpb", [128, 2, D], BF16)
            d_w = S.dsem("w")
            for k in range(8):
                S.dma("pool", winb[:, k, :], w_in[k * 128:(k + 1) * 128, :], d_w, writes=["winb"])
            S.dma("pool", woutb[:], w_out.rearrange("(k p) n -> p k n", p=128), d_w, writes=["woutb"])
            S.dma("sp", wr[:], w_router.rearrange("(k p) n -> p k n", p=128), d_misc, writes=["wr"])
            S.dma("pool", wsb[:, :, 0:256], ws1.rearrange("(k p) n -> p k n", p=128), d_w, writes=["wsb"])
            S.dma("pool", wsb[:, :, 256:512], ws3.rearrange("(k p) n -> p k n", p=128), d_w, writes=["wsb"])
            S.dma("pool", ws2b[:], ws2.rearrange("(k p) n -> p k n", p=128), d_w, writes=["ws2b"])
            S.dma("pool", wgb[:], w_pg.rearrange("(k p) n -> p k n", p=128), d_w, writes=["wgb"])
            S.dma("pool", wpb[:], w_pp.rearrange("(k p) n -> p k n", p=128), d_w, writes=["wpb"])
            identf = sb("identf", [128, 128]); dtm = sb("dtm", [128, 8, 128]); qw = sb("qw", [128, 4, 128])
            kwt = sb("kwt", [128, 8]); cdt = sb("cdt", [128, 4, 64])
            mcur = sb("mcur", [128, 128], BF16); mprev = sb("mprev", [128, 128], BF16)
            utri = sb("utri", [128, 128], BF16); onesb = sb("onesb", [128, 128], BF16)
            eoff = sb("eoff", [128, NE]); tokc = sb("tokc", [128, NCH], I32)
            gain = sb("gain", [128, 512]); ascale = sb("ascale", [128, 512]); esink = sb("esink", [128, 8])
            ln1g = sb("ln1g", [128, D]); ln1b = sb("ln1b", [128, D]); bgb = sb("bgb", [1, D], BF16)
            rbias = sb("rbias", [128, NE]); neghalf = sb("neghalf", [128, 8])
            d_c = S.dsem("c"); d_cp = S.dsem("cp")
            for (t, src, q) in ((identf, c_ident, "sp"), (dtm, c_dt.rearrange("p (h q) -> p h q", q=128), "sp"),
                                (qw, c_qw.rearrange("p (j q) -> p j q", q=128), "sp"), (kwt, c_kw, "sp"),
                                (cdt, c_cd.rearrange("p (j e) -> p j e", e=64), "sp"),
                                (mcur, c_mcur, "pool"), (mprev, c_mprev, "pool"), (utri, c_utri, "pool"),
                                (onesb, c_ones, "pool"), (eoff, c_eoff, "sp"), (tokc, c_tok, "sp"),
                                (gain, gain_r, "sp"), (ascale, ascale_r, "sp"), (esink, sinks_r, "sp"),
                                (ln1g, ln1g_r, "sp"), (ln1b, ln1b_r, "sp"), (bgb, bgate_r[0:1, :], "pool"),
                                (rbias, rbias_r, "sp")):
                S.dma(q, t[:], src, d_c if q == "sp" else d_cp)
            for e in ("pe", "act", "dve", "pool"):
                for dk in (d_c, d_cp, d_w, d_misc, d_miscp):
                    S._need(e, dk, S.cnt[dk])
            S.op("act", L("activation", out=esink[:], in_=esink[:], func=AF.Exp), writes=["esink"])
            S.op("dve", L("memset", neghalf[:], -0.5), writes=["neghalf"])

            xf = [sb(f"xf{i}", [128, D]) for i in range(2)]
            pf = [sb(f"pf{i}", [128, 256]) for i in range(2)]
            rope = [sb(f"rope{i}", [128, 160]) for i in range(2)]
            xb = sb("xb", [128, D], BF16); xT = sb("xT", [128, 8, 128], BF16)
            pb = sb("pb", [128, 256], BF16); ppT_ = [sb(f"ppT{i_}", [128, 2, 128], BF16) for i_ in range(2)]
            ropA = sb("ropA", [128, 8, 64]); ropB = sb("ropB", [128, 8, 64])
            rq_rot = sb("rq_rot", [128, 8, 64], BF16); rk_rot = sb("rk_rot", [128, 8, 64], BF16)
            kw = sb("kw", [128, 8, 64], BF16); rv = sb("rv", [128, 512], BF16); sg = sb("sg", [128, 512])
            aq_st = sb("aq_st", [128, 8, 64], BF16); ak_rot = sb("ak_rot", [128, 2, 64], BF16); akvf = sb("akvf", [128, 256])
            vaug = [sb(f"vaug{i}", [128, 2, 66], BF16) for i in range(2)]
            akT = [sb(f"akT{i}", [128, 128], BF16) for i in range(2)]
            rqkT = sb("rqkT", [128, 8, 128], BF16); rqTw = sb("rqTw", [128, 4, 128], BF16)
            aqT = sb("aqT", [128, 4, 128], BF16)
            sd = sb("sd", [128, 8, 128], BF16)
            state = sb("state", [128, 4, 64]); sttmp = sb("sttmp", [128, 4, 64]); stateb = sb("stateb", [128, 4, 64], BF16)
            gs = sb("gs", [128, 512])
            st1 = sb("st1", [128, 8]); st2 = sb("st2", [128, 8]); stm = sb("stm", [128, 8]); stv = sb("stv", [128, 8])
            strs = sb("strs", [128, 8])
            eT = [sb(f"eT{i}", [128, 2, 512], BF16) for i in range(2)]
            den = sb("den", [128, 8]); rden = sb("rden", [128, 8]); att_t = sb("att_t", [128, 8, 64])
            mixcat_ = [sb(f"mixcat{i_}", [128, D], BF16) for i_ in range(2)]; mixT = sb("mixT", [128, 8, 128], BF16)
            bigA = sb("bigA", [128, D]); x1 = sb("x1", [128, D]); bigC = sb("bigC", [128, D])
            bnst = sb("bnst", [128, 2, 6]); mv = sb("mv", [128, 2]); rstd = sb("rstd", [128, 1]); nmr = sb("nmr", [128, 1])
            x1Tf = sb("x1Tf", [128, 8, 128]); x1Tb = sb("x1Tb", [128, 8, 128], BF16)
            sl = sb("sl", [128, 2, 128]); hsT = sb("hsT", [128, 2, 128], BF16)
            scores = sb("scores", [128, NE]); biased = sb("biased", [128, NE]); r_eq = sb("r_eq", [128, NE])
            r_b2 = sb("r_b2", [128, NE]); r_m1 = sb("r_m1", [128, 8]); r_m2 = sb("r_m2", [128, 8]); r_gs = sb("r_gs", [128, 8])
            r_gtop = sb("r_gtop", [128, 8]); r_gm = sb("r_gm", [128, 8]); r_neg = sb("r_neg", [128, 8])
            r_msk = sb("r_msk", [128, NE]); r_top = sb("r_top", [128, 8]); r_M = sb("r_M", [128, NE]); r_Mb = sb("r_Mb", [128, NE], BF16)
            r_w = sb("r_w", [128, NE]); r_ws = sb("r_ws", [128, 1]); r_rs = sb("r_rs", [128, 1]); cm = sb("cm", [128, NE])
            r_pos = sb("r_pos", [128, NE]); r_run = sb("r_run", [128, NE]); r_key = sb("r_key", [128, NE]); r_val = sb("r_val", [128, NE])
            r_k8 = sb("r_k8", [128, 8]); r_z = sb("r_z", [128, 8]); r_f8 = sb("r_f8", [128, 8])
            TB0 = ps("TB0", [128, 8, 128], BF16); TB1 = ps("TB1", [128, 16, 128], BF16)
            Fb = [ps(f"F{i}", [128, 512]) for i in range(5)]

            tli = sb("tli", [128, NE * CAP // 128], I32)
            S.op("dve", L("memset", tli[:], DUMMY_TOK), writes=["tli"])
            S.op("dve", L("memset", bigC[:], 0.0), writes=["bigC"])
            S.op("dve", L("memset", xb[:], 0.0), writes=["xb"])
            S.dma("sp", tl[0:NE * CAP, :].rearrange("(p f) o -> p (f o)", p=128), tli[:], d_init, reads=["tli"], writes=["tl"])
            S.dma("sp", tl[NE * CAP:NE * CAP + 128, :], tli[:, 0:1], d_init, reads=["tli"], writes=["tl"])
            S.dma("sp", x1d[S_LEN:S_LEN + 1, :], xb[0:1, :], d_init, reads=["xb"], writes=["x1d_z"])
            S.dma("sp", cmatd[S_LEN:S_LEN + 1, :], bigC[0:1, 0:NE], d_init, reads=["bigC"], writes=["cmatd_z"])
            S.dma("sp", Yd[DUMMY_SLOT:DUMMY_SLOT + 1, :], bigC[0:1, :], d_init, reads=["bigC"], writes=["Yd_z"])
            for e in ("pe", "act", "dve", "pool", "sp"):
                S._need(e, d_init, S.cnt[d_init])

            S.op("dve", L("memset", state[:], 0.0), writes=["state"])
            S.op("dve", L("memset", stateb[:], 0.0), writes=["stateb"])
            S.op("dve", L("memset", r_run[:], 0.0), writes=["r_run"])
            for i in range(2):
                S.op("dve", L("memset", vaug[i][:], 1.0), writes=[f"vaug{i}"])

            d_x = [S.dsem(f"x{i}") for i in range(2)]
            d_p = [S.dsem(f"p{i}") for i in range(2)]
            d_r = [S.dsem(f"r{i}") for i in range(2)]
            d_st_x = S.dsem("stx"); d_st_b = S.dsem("stb"); d_st_c = S.dsem("stc")
            d_sc = S.dsem("sc")

            def load_chunk(n):
                i = n % 2
                S.dma("sp", xf[i][:], x[n * 128:(n + 1) * 128, :], d_x[i], writes=[f"xf{i}"])
                S.dma("sp", pf[i][:], p_in[n * 128:(n + 1) * 128, :], d_p[i], writes=[f"pf{i}"])
                S.dma("sp", rope[i][:], c_rope[n], d_r[i], writes=[f"rope{i}"])

            def rope_apply(src, H, half, cc, ss, dst, key_src, key_dst, rkey, Hout=None):
                w2_ = 2 * half
                nops = int(os.environ.get("K_ROPE_N", "4")) if half == 8 else 4
                if nops >= 1:
                    S.op("dve", L("tensor_tensor", out=ropA[:, 0:H, 0:w2_], in0=src[:, :, 0:w2_],
                                  in1=bc(cc, [128, H, w2_], 1), op=ALU.mult),
                         reads=[key_src, rkey], writes=["ropA"])
                if nops >= 2:
                    S.op("dve", L("tensor_tensor", out=ropB[:, 0:H, 0:half], in0=src[:, :, half:w2_],
                                  in1=bc(ss[:, 0:half], [128, H, half], 1), op=ALU.mult),
                         reads=[key_src, rkey], writes=["ropB"])
                if nops >= 3:
                    S.op("dve", L("tensor_tensor", out=ropB[:, 0:H, half:w2_], in0=src[:, :, 0:half],
                                  in1=bc(ss[:, half:w2_], [128, H, half], 1), op=ALU.mult),
                         reads=[key_src, rkey], writes=["ropB"])
                Ho = Hout or H
                if nops >= 4:
                    S.op(ROPE_ADD_ENG, L("tensor_tensor", out=dst, in0=ropA[:, 0:Ho, 0:w2_], in1=ropB[:, 0:Ho, 0:w2_], op=ALU.add),
                         reads=["ropA", "ropB"], writes=[key_dst])

            load_chunk(0)
            def chunk(n):
                i = n % 2
                rk_ = f"rope{i}"
                rp = rope[i]
                S.op("act", L("copy", out=xb[:], in_=xf[i][:]), reads=[f"xf{i}"], writes=["xb"])
                S.op("act", L("copy", out=pb[:], in_=pf[i][:]), reads=[f"pf{i}"], writes=["pb"])
                for k in range(8):
                    S.op("pe", L("transpose", out=TB0[:, k, :], in_=xb[:, k * 128:(k + 1) * 128], identity=identb[:]),
                         reads=["xb"], writes=["TB0"], inc=(k == 7))
                S.op("act", L("copy", out=xT[:], in_=TB0[:]), reads=["TB0"], writes=["xT"])
                yield None
                def proj(g, bank):
                    c0 = g * 512
                    wdt = min(512, 2816 - c0)
                    for k in range(8):
                        S.op("pe", L("matmul", Fb[bank][:, 0:wdt], lhsT=xT[:, k, :], rhs=winb[:, k, c0:c0 + wdt],
                                                       start=(k == 0), stop=(k == 7)),
                             reads=["xT"], writes=[f"F{bank}"], inc=(k == 7))
                proj(0, 0); proj(1, 1); proj(2, 2)
                rope_apply(Fb[0][:].rearrange("p (h d) -> p h d", d=64), 8, 32, rp[:, 0:64], rp[:, 64:128],
                           rq_rot[:], "F0", "rq_rot", rk_)
                proj(3, 0)
                rope_apply(Fb[1][:].rearrange("p (h d) -> p h d", d=64), 8, 32, rp[:, 0:64], rp[:, 64:128],
                           rk_rot[:], "F1", "rk_rot", rk_)
                S.op("dve", L("tensor_tensor", out=kw[:], in0=rk_rot[:], in1=bc(kwt[:, :], [128, 8, 64], 2), op=ALU.mult),
                     reads=["rk_rot"], writes=["kw"])
                proj(4, 1)
                S.op("act", L("copy", out=rv[:], in_=Fb[2][:]), reads=["F2"], writes=["rv"])
                proj(5, 2)
                S.op("act", L("activation", out=sg[:], in_=Fb[0][:], func=AF.Silu), reads=["F0"], writes=["sg"])
                aq_src = Fb[1][:].rearrange("p (k g d) -> p k g d", k=2, g=4)
                aq_dst = aq_st[:].rearrange("p (g k) d -> p k g d", k=2)
                S.op("act", L("copy", out=aq_dst, in_=aq_src), reads=["F1"], writes=["aq_st"])
                for hk in range(2):
                    rope_apply(aq_src[:, hk], 4, 8, rp[:, 128:144], rp[:, 144:160],
                               aq_dst[:, hk, :, 0:16], "F1", "aq_st", rk_)
                ak_src = Fb[2][:, 0:128].rearrange("p (h d) -> p h d", d=64)
                S.op("act", L("copy", out=ak_rot[:], in_=ak_src), reads=["F2"], writes=["ak_rot"])
                S.op("act", L("copy", out=akvf[:], in_=Fb[2][:, 0:256]), reads=["F2"], writes=["akvf"])
                if SKIP_AK_ROPE:
                    pass
                else:
                    rope_apply(akvf[:].rearrange("p (h d) -> p h d", d=64), 4, 8, rp[:, 128:144], rp[:, 144:160], ak_rot[:, :, 0:16], "akvf", "ak_rot", rk_, Hout=2)
                S.op("act", L("copy", out=vaug[i][:, :, 0:64], in_=Fb[2][:, 128:256].rearrange("p (h d) -> p h d", d=64)),
                     reads=["F2"], writes=[f"vaug{i}"])
                yield None
                if n + 1 < nrun:
                    load_chunk(n + 1)
                for j in range(4):
                    S.op("pe", L("transpose", out=TB1[:, j, :], in_=rq_rot[:].rearrange("p h d -> p (h d)")[:, j * 128:(j + 1) * 128],
                                                      identity=identb[:]), reads=["rq_rot"], writes=["TB1a"], inc=False)
                for j in range(4):
                    S.op("pe", L("transpose", out=TB1[:, 4 + j, :], in_=rk_rot[:].rearrange("p h d -> p (h d)")[:, j * 128:(j + 1) * 128],
                                                      identity=identb[:]), reads=["rk_rot"], writes=["TB1a"], inc=(j == 3))
                S.op("act", L("copy", out=rqkT[:], in_=TB1[:, 0:8, :]), reads=["TB1a"], writes=["rqkT"])
                S.op("dve", L("tensor_tensor", out=rqTw[:], in0=TB1[:, 0:4, :], in1=qw[:], op=ALU.mult),
                     reads=["TB1a"], writes=["rqTw"])
                for j in range(4):
                    S.op("pe", L("transpose", out=TB1[:, 8 + j, :], in_=aq_st[:].rearrange("p h d -> p (h d)")[:, j * 128:(j + 1) * 128],
                                                      identity=identb[:]), reads=["aq_st"], writes=["TB1b"], inc=False)
                S.op("pe", L("transpose", out=TB1[:, 12, :], in_=ak_rot[:].rearrange("p h d -> p (h d)"), identity=identb[:]),
                     reads=["ak_rot"], writes=["TB1b"], inc=False)
                for c in range(2):
                    S.op("pe", L("transpose", out=TB1[:, 13 + c, :], in_=pb[:, c * 128:(c + 1) * 128], identity=identb[:]),
                         reads=["pb"], writes=["TB1b"], inc=(c == 1))
                S.op("act", L("copy", out=aqT[:], in_=TB1[:, 8:12, :]), reads=["TB1b"], writes=["aqT"])
                S.op("dve", L("tensor_copy", out=akT[i][:], in_=TB1[:, 12, :]), reads=["TB1b"], writes=[f"akT{i}"])
                S.op("dve", L("tensor_copy", out=ppT_[i][:], in_=TB1[:, 13:15, :]), reads=["TB1b"], writes=[f"ppT{i}"])
                yield None
                for h in range(8):
                    hp, j = h % 2, h // 2
                    bank = 3 + hp
                    S.op("pe", L("matmul", Fb[bank][:, j * 128:(j + 1) * 128],
                                 lhsT=rqkT[hp * 64:hp * 64 + 64, 4 + j, :], rhs=rqkT[hp * 64:hp * 64 + 64, j, :],
                                 start=True, stop=True),
                         reads=["rqkT"], writes=[f"F{bank}"], inc=(h >= 6))
                for hp in range(2):
                    S.op("dve", L("tensor_tensor", out=sd[:].rearrange("p (j t) q -> p t j q", t=2)[:, hp],
                                  in0=Fb[3 + hp][:].rearrange("p (j q) -> p j q", q=128),
                                  in1=dtm[:].rearrange("p (j t) q -> p t j q", t=2)[:, hp], op=ALU.mult),
                         reads=[f"F{3 + hp}"], writes=["sd"])
                for h in range(8):
                    hp, j = h % 2, h // 2
                    S.op("pe", L("matmul", Fb[0][:, h * 64:(h + 1) * 64], lhsT=sd[:, h, :], rhs=rv[:, h * 64:(h + 1) * 64],
                                                   start=True, stop=False), reads=["sd", "rv"], writes=["F0"], inc=False)
                    S.op("pe", L("matmul", Fb[0][:, h * 64:(h + 1) * 64], lhsT=rqTw[hp * 64:hp * 64 + 64, j, :],
                                                   rhs=stateb[hp * 64:hp * 64 + 64, j, :], start=False, stop=True),
                         reads=["rqTw", "stateb"], writes=["F0"], inc=(h == 7))
                for h in range(8):
                    hp, j = h % 2, h // 2
                    S.op("pe", L("matmul", Fb[1][hp * 64:hp * 64 + 64, j * 64:(j + 1) * 64], lhsT=kw[:, h, :],
                                                   rhs=rv[:, h * 64:(h + 1) * 64], start=True, stop=True),
                         reads=["kw", "rv"], writes=["F1"], inc=(h == 7))
                S.op("dve", L("tensor_tensor", out=sttmp[:], in0=state[:], in1=cdt[:], op=ALU.mult),
                     reads=["state"], writes=["sttmp"])
                S.op("dve", L("tensor_tensor", out=state[:], in0=sttmp[:], in1=Fb[1][:, 0:256].rearrange("p (j e) -> p j e", e=64),
                                                      op=ALU.add), reads=["sttmp", "F1"], writes=["state"])
                S.op("act", L("copy", out=stateb[:], in_=state[:]), reads=["state"], writes=["stateb"])
                pO3 = Fb[0][:].rearrange("p (h d) -> p h d", d=64)
                S.op("dve", L("tensor_reduce", out=st1[:], in_=pO3, axis=AX.X, op=ALU.add), reads=["F0"], writes=["st1"])
                S.op("act", L("activation", out=att_t[:].rearrange("p h d -> p (h d)"), in_=Fb[0][:], func=AF.Square), reads=["F0"], writes=["att_t"])
                S.op("dve", L("tensor_reduce", out=st2[:], in_=att_t[:], axis=AX.X, op=ALU.add),
                     reads=["att_t"], writes=["st2"])
                S.op("dve", L("tensor_scalar", out=stm[:], in0=st1[:], scalar1=1.0 / 64, scalar2=None, op0=ALU.mult),
                     reads=["st1"], writes=["stm"])
                S.op("dve", L("tensor_tensor", out=stv[:], in0=stm[:], in1=stm[:], op=ALU.mult), reads=["stm"], writes=["stv"])
                S.op("dve", L("scalar_tensor_tensor", out=stv[:], in0=st2[:], scalar=1.0 / 64, in1=stv[:], op0=ALU.mult, op1=ALU.subtract),
                     reads=["st2", "stv"], writes=["stv"])
                S.op("dve", L("tensor_scalar", out=stv[:], in0=stv[:], scalar1=1e-6, scalar2=None, op0=ALU.add),
                     reads=["stv"], writes=["stv"])
                S.op("act", L("activation", out=strs[:], in_=stv[:], func=AF.Ln), reads=["stv"], writes=["strs"])
                S.op("act", L("activation", out=strs[:], in_=strs[:], func=AF.Exp, scale=-0.5), reads=["strs"], writes=["strs"])
                S.op("dve", L("tensor_tensor", out=ropA[:], in0=pO3, in1=bc(stm[:, :], [128, 8, 64], 2), op=ALU.subtract),
                     reads=["F0", "stm"], writes=["ropA"])
                S.op("dve", L("tensor_tensor", out=gs[:], in0=sg[:], in1=gain[:], op=ALU.mult), reads=["sg"], writes=["gs"])
                S.op("dve", L("tensor_tensor", out=ropA[:], in0=ropA[:], in1=bc(strs[:, :], [128, 8, 64], 2), op=ALU.mult),
                     reads=["ropA", "strs"], writes=["ropA"])
                if os.environ.get("K_ALT") == "1":
                    S.op("dve", L("memset", mixcat_[i][:, 0:512], 0.0), reads=["ropA", "gs"], writes=[f"mixcat{i}"])
                elif os.environ.get("K_ALT") == "2":
                    S.op("dve", L("tensor_tensor", out=bigA[:, 0:512], in0=ropA[:].rearrange("p h d -> p (h d)"), in1=gs[:], op=ALU.mult),
                         reads=["ropA", "gs"], writes=[f"mixcat{i}"])
                else:
                    S.op("dve", L("tensor_tensor", out=mixcat_[i][:, 0:512], in0=ropA[:].rearrange("p h d -> p (h d)"), in1=gs[:], op=ALU.mult),
                         reads=["ropA", "gs"], writes=[f"mixcat{i}"])
                yield None
                blks = ([(1 - i, mprev, 0)] if n > 0 else []) + [(i, mcur, 1)]
                cnt_as = 0
                for (par, msk, bi) in blks:
                    for hk in range(2):
                        bank = 3 + (cnt_as % 2); cnt_as += 1
                        S.op("pe", L("matmul", Fb[bank][:], lhsT=akT[par][hk * 64:hk * 64 + 64, :],
                                                       rhs=aqT[hk * 64:hk * 64 + 64, :, :].rearrange("p g q -> p (g q)"),
                                                       start=True, stop=True),
                             reads=[f"akT{par}", "aqT"], writes=[f"F{bank}"])
                        S.op("act", L("activation", out=eT[bi][:, hk, :], in_=Fb[bank][:], func=AF.Exp, scale=0.125),
                             reads=[f"F{bank}"], writes=[f"eT{bi}"])
                    S.op("dve", L("tensor_tensor", out=eT[bi][:].rearrange("p k (g q) -> p (k g) q", q=128),
                                                           in0=eT[bi][:].rearrange("p k (g q) -> p (k g) q", q=128),
                                                           in1=bc(msk[:, :], [128, 8, 128], 1), op=ALU.mult),
                         reads=[f"eT{bi}"], writes=[f"eT{bi}"])
                pv_bank = {0: 2, 1: 1}
                for hk in range(2):
                    bank = pv_bank[hk]
                    for g in range(4):
                        for bidx, (par, msk, bi) in enumerate(blks):
                            S.op("pe", L("matmul", Fb[bank][:, g * 66:(g + 1) * 66], lhsT=eT[bi][:, hk, g * 128:(g + 1) * 128],
                                                           rhs=vaug[par][:, hk, :], start=(bidx == 0), stop=(bidx == len(blks) - 1)),
                                 reads=[f"eT{bi}", f"vaug{par}"], writes=[f"F{bank}"], inc=(g == 3 and bidx == len(blks) - 1))
                for hk in range(2):
                    bank = pv_bank[hk]
                    v4 = Fb[bank][:, 0:264].rearrange("p (g c) -> p g c", c=66)
                    S.op("dve", L("tensor_tensor", out=den[:, hk * 4:(hk + 1) * 4], in0=v4[:, :, 64], in1=esink[:, hk * 4:(hk + 1) * 4], op=ALU.add),
                         reads=[f"F{bank}", "esink"], writes=["den"])
                S.op("dve", L("reciprocal", out=rden[:], in_=den[:]), reads=["den"], writes=["rden"])
                for hk in range(2):
                    bank = pv_bank[hk]
                    v4 = Fb[bank][:, 0:264].rearrange("p (g c) -> p g c", c=66)
                    S.op("dve", L("tensor_tensor", out=att_t[:, hk * 4:(hk + 1) * 4, :], in0=v4[:, :, 0:64],
                                                          in1=bc(rden[:, hk * 4:(hk + 1) * 4], [128, 4, 64], 2), op=ALU.mult),
                         reads=[f"F{bank}", "rden"], writes=["att_t"])
                S.op("dve", L("tensor_tensor", out=mixcat_[i][:, 512:1024], in0=att_t[:].rearrange("p h d -> p (h d)"), in1=ascale[:], op=ALU.mult),
                     reads=["att_t"], writes=[f"mixcat{i}"])
                yield "HALF"
                for k in range(8):
                    S.op("pe", L("transpose", out=TB0[:, k, :], in_=mixcat_[i][:, k * 128:(k + 1) * 128], identity=identb[:]),
                         reads=[f"mixcat{i}"], writes=["TB0"], inc=(k == 7))
                S.op("act", L("copy", out=mixT[:], in_=TB0[:]), reads=["TB0"], writes=["mixT"])
                for hf in range(2):
                    for k in range(8):
                        S.op("pe", L("matmul", Fb[3 + hf][:], lhsT=mixT[:, k, :], rhs=woutb[:, k, hf * 512:(hf + 1) * 512],
                                                       start=(k == 0), stop=(k == 7)),
                             reads=["mixT"], writes=[f"F{3 + hf}"], inc=(k == 7))
                for hf in range(2):
                    S.op("dve", L("scalar_tensor_tensor", out=bigA[:, hf * 512:(hf + 1) * 512], in0=xf[i][:, hf * 512:(hf + 1) * 512],
                                                                 scalar=ALPHA, in1=Fb[3 + hf][:], op0=ALU.mult, op1=ALU.add),
                         reads=[f"xf{i}", f"F{3 + hf}"], writes=["bigA"])
                for hf in range(2):
                    S.op("dve", L("bn_stats", out=bnst[:, hf, :], in_=bigA[:, hf * 512:(hf + 1) * 512]), reads=["bigA"], writes=["bnst"])
                S.op("dve", L("bn_aggr", out=mv[:], in_=bnst[:].rearrange("p a b -> p (a b)")), reads=["bnst"], writes=["mv"])
                S.op("dve", L("tensor_scalar", out=rstd[:], in0=mv[:, 1:2], scalar1=1e-5, scalar2=None, op0=ALU.add),
                     reads=["mv"], writes=["rstd"])
                S.op("act", L("activation", out=rstd[:], in_=rstd[:], func=AF.Ln), reads=["rstd"], writes=["rstd"])
                S.op("act", L("activation", out=rstd[:], in_=rstd[:], func=AF.Exp, scale=-0.5), reads=["rstd"], writes=["rstd"])
                S.op("dve", L("scalar_tensor_tensor", out=nmr[:], in0=mv[:, 0:1], scalar=-1.0, in1=rstd[:], op0=ALU.mult, op1=ALU.mult),
                     reads=["mv", "rstd"], writes=["nmr"])
                S.op("act", L("activation", out=x1[:], in_=bigA[:], func=AF.Identity, bias=nmr[:, 0:1], scale=rstd[:, 0:1]),
                     reads=["bigA", "nmr", "rstd"], writes=["x1"])
                S.op("dve", L("tensor_tensor", out=x1[:], in0=x1[:], in1=ln1g[:], op=ALU.mult), reads=["x1"], writes=["x1"])
                S.op("dve", L("tensor_tensor", out=x1[:], in0=x1[:], in1=ln1b[:], op=ALU.add), reads=["x1"], writes=["x1"])
                yield None
                S.op("act", L("copy", out=xb[:], in_=x1[:]), reads=["x1"], writes=["xb"])
                S.dma("sp", x1d[n * 128:(n + 1) * 128, :], xb[:], d_st_x, reads=["xb"], writes=[f"x1d{n}"])
                x1T_ps = [Fb[0][:].rearrange("p (k t) -> p k t", t=128), Fb[1][:].rearrange("p (k t) -> p k t", t=128)]
                for k in range(8):
                    S.op("pe", L("transpose", out=x1T_ps[k // 4][:, k % 4, :], in_=x1[:, k * 128:(k + 1) * 128], identity=identf[:]),
                         reads=["x1"], writes=[f"F{k // 4}"], inc=(k % 4 == 3))
                for hf in range(2):
                    S.op("dve", L("tensor_copy", out=x1Tf[:, hf * 4:(hf + 1) * 4, :], in_=x1T_ps[hf]), reads=[f"F{hf}"], writes=["x1Tf"])
                    S.op("act", L("copy", out=x1Tb[:, hf * 4:(hf + 1) * 4, :], in_=x1T_ps[hf]), reads=[f"F{hf}"], writes=["x1Tb"])
                for k in range(8):
                    S.op("pe", L("matmul", Fb[2][:, 0:NE], lhsT=x1Tf[:, k, :], rhs=wr[:, k, :], start=(k == 0), stop=(k == 7)),
                         reads=["x1Tf"], writes=["F2"], inc=(k == 7))
                S.op("act", L("activation", out=scores[:], in_=Fb[2][:, 0:NE], func=AF.Sigmoid), reads=["F2"], writes=["scores"])
                hps = Fb[3][:].rearrange("p (m t) -> p m t", t=128)
                for m in range(4):
                    for k in range(8):
                        S.op("pe", L("matmul", hps[:, m, :], lhsT=wsb[:, k, m * 128:(m + 1) * 128], rhs=x1Tb[:, k, :],
                                                       start=(k == 0), stop=(k == 7)),
                             reads=["x1Tb"], writes=["F3"], inc=(m == 3 and k == 7))
                S.op("act", L("activation", out=sl[:], in_=hps[:, 0:2, :], func=AF.Silu), reads=["F3"], writes=["sl"])
                S.op("dve", L("tensor_tensor", out=hsT[:], in0=sl[:], in1=hps[:, 2:4, :], op=ALU.mult), reads=["sl", "F3"], writes=["hsT"])
                for hf in range(2):
                    for c in range(2):
                        S.op("pe", L("matmul", Fb[hf][:], lhsT=hsT[:, c, :], rhs=ws2b[:, c, hf * 512:(hf + 1) * 512],
                                                       start=(c == 0), stop=(c == 1)),
                             reads=["hsT"], writes=[f"F{hf}"], inc=(c == 1))
                for hf in range(2):
                    for k in range(8):
                        S.op("pe", L("matmul", Fb[3 + hf][:], lhsT=x1Tb[:, k, :], rhs=wgb[:, k, hf * 512:(hf + 1) * 512],
                                                       start=(k == 0), stop=False),
                             reads=["x1Tb"], writes=[f"F{3 + hf}"], inc=False)
                    S.op("pe", L("matmul", Fb[3 + hf][:], lhsT=onesb[0:1, :], rhs=bgb[0:1, hf * 512:(hf + 1) * 512], start=False, stop=True),
                         reads=["x1Tb"], writes=[f"F{3 + hf}"])
                for hf in range(2):
                    S.op("act", L("activation", out=bigA[:, hf * 512:(hf + 1) * 512], in_=Fb[3 + hf][:], func=AF.Sigmoid),
                         reads=[f"F{3 + hf}"], writes=["bigA"])
                for hf in range(2):
                    for c in range(2):
                        S.op("pe", L("matmul", Fb[3 + hf][:], lhsT=ppT_[i][:, c, :], rhs=wpb[:, c, hf * 512:(hf + 1) * 512],
                                                       start=(c == 0), stop=(c == 1)),
                             reads=[f"ppT{i}"], writes=[f"F{3 + hf}"], inc=(c == 1))
                for hf in range(2):
                    sl_ = slice(hf * 512, (hf + 1) * 512)
                    S.op("dve", L("tensor_tensor", out=bigC[:, sl_], in0=bigA[:, sl_], in1=Fb[3 + hf][:], op=ALU.mult),
                         reads=["bigA", f"F{3 + hf}"], writes=["bigC"])
                S.op("dve", L("scalar_tensor_tensor", out=bigC[:], in0=x1[:], scalar=ALPHA, in1=bigC[:], op0=ALU.mult, op1=ALU.add),
                     reads=["x1", "bigC"], writes=["bigC"])
                for hf in range(2):
                    sl_ = slice(hf * 512, (hf + 1) * 512)
                    S.op("dve", L("tensor_tensor", out=bigC[:, sl_], in0=bigC[:, sl_], in1=Fb[hf][:], op=ALU.add),
                         reads=["bigC", f"F{hf}"], writes=["bigC"])
                S.dma("sp", based[n * 128:(n + 1) * 128, :], bigC[:], d_st_b, reads=["bigC"], writes=[f"based{n}"])
                yield None
                V = lambda fn, r, w: S.op("dve", fn, reads=r, writes=w)
                V(L("tensor_tensor", out=biased[:], in0=scores[:], in1=rbias[:], op=ALU.add), ["scores"], ["biased"])
                b3 = biased[:].rearrange("p (g k) -> p g k", k=8)
                V(L("tensor_reduce", out=r_m1[:], in_=b3, axis=AX.X, op=ALU.max), ["biased"], ["r_m1"])
                V(L("tensor_tensor", out=r_eq[:].rearrange("p (g k) -> p g k", k=8), in0=b3, in1=bc(r_m1[:, :], [128, 8, 8], 2), op=ALU.is_equal),
                  ["biased", "r_m1"], ["r_eq"])
                V(L("scalar_tensor_tensor", out=r_b2[:], in0=r_eq[:], scalar=-1e30, in1=biased[:], op0=ALU.mult, op1=ALU.add),
                  ["r_eq", "biased"], ["r_b2"])
                V(L("tensor_reduce", out=r_m2[:], in_=r_b2[:].rearrange("p (g k) -> p g k", k=8), axis=AX.X, op=ALU.max), ["r_b2"], ["r_m2"])
                V(L("tensor_tensor", out=r_gs[:], in0=r_m1[:], in1=r_m2[:], op=ALU.add), ["r_m1", "r_m2"], ["r_gs"])
                V(L("max", out=r_gtop[:], in_=r_gs[:]), ["r_gs"], ["r_gtop"])
                V(L("tensor_scalar", out=r_gm[:], in0=r_gs[:], scalar1=r_gtop[:, 3:4], scalar2=None, op0=ALU.is_ge), ["r_gs", "r_gtop"], ["r_gm"])
                V(L("tensor_scalar", out=r_neg[:], in0=r_gm[:], scalar1=1e30, scalar2=-1e30, op0=ALU.mult, op1=ALU.add), ["r_gm"], ["r_neg"])
                m3 = r_msk[:].rearrange("p (g k) -> p g k", k=8)
                V(L("tensor_tensor", out=m3, in0=b3, in1=bc(r_gm[:, :], [128, 8, 8], 2), op=ALU.mult), ["biased", "r_gm"], ["r_msk"])
                V(L("tensor_tensor", out=m3, in0=m3, in1=bc(r_neg[:, :], [128, 8, 8], 2), op=ALU.add), ["r_msk", "r_neg"], ["r_msk"])
                V(L("max", out=r_top[:], in_=r_msk[:]), ["r_msk"], ["r_top"])
                V(L("tensor_scalar", out=r_M[:], in0=r_msk[:], scalar1=r_top[:, 7:8], scalar2=None, op0=ALU.is_ge), ["r_msk", "r_top"], ["r_M"])
                V(L("tensor_tensor", out=r_w[:], in0=scores[:], in1=r_M[:], op=ALU.mult), ["scores", "r_M"], ["r_w"])
                V(L("tensor_reduce", out=r_ws[:], in_=r_w[:], axis=AX.X, op=ALU.add), ["r_w"], ["r_ws"])
                V(L("reciprocal", out=r_rs[:], in_=r_ws[:]), ["r_ws"], ["r_rs"])
                V(L("tensor_scalar", out=cm[:], in0=r_w[:], scalar1=r_rs[:, 0:1], scalar2=2.5, op0=ALU.mult, op1=ALU.mult), ["r_w", "r_rs"], ["cm"])
                S.dma("sp", cmatd[n * 128:(n + 1) * 128, :], cm[:], d_st_c, reads=["cm"], writes=[f"cmatd{n}"])
                S.op("act", L("copy", out=r_Mb[:], in_=r_M[:]), reads=["r_M"], writes=["r_Mb"])
                S.op("pe", L("matmul", Fb[2][:, 64:128], lhsT=utri[:], rhs=r_Mb[:], start=True, stop=True), reads=["r_Mb"], writes=["F2"], inc=False)
                S.op("pe", L("matmul", Fb[2][:, 128:192], lhsT=onesb[:], rhs=r_Mb[:], start=True, stop=True), reads=["r_Mb"], writes=["F2"])
                V(L("tensor_tensor", out=r_pos[:], in0=Fb[2][:, 64:128], in1=r_run[:], op=ALU.add), ["F2", "r_run"], ["r_pos"])
                V(L("tensor_tensor", out=r_run[:], in0=Fb[2][:, 128:192], in1=r_run[:], op=ALU.add), ["F2", "r_run"], ["r_run"])
                V(L("tensor_scalar", out=r_val[:], in0=r_pos[:], scalar1=float(CAP), scalar2=None, op0=ALU.is_lt), ["r_pos"], ["r_val"])
                V(L("tensor_tensor", out=r_key[:], in0=r_pos[:], in1=eoff[:], op=ALU.add), ["r_pos"], ["r_key"])
                V(L("tensor_tensor", out=r_key[:], in0=r_key[:], in1=r_M[:], op=ALU.mult), ["r_key", "r_M"], ["r_key"])
                V(L("tensor_tensor", out=r_key[:], in0=r_key[:], in1=r_val[:], op=ALU.mult), ["r_key", "r_val"], ["r_key"])
                V(L("max", out=r_k8[:], in_=r_key[:]), ["r_key"], ["r_k8"])
                V(L("tensor_scalar", out=r_z[:], in0=r_k8[:], scalar1=0.5, scalar2=float(DUMMY_SLOT + 1), op0=ALU.is_lt, op1=ALU.mult), ["r_k8"], ["r_z"])
                V(L("scalar_tensor_tensor", out=r_f8[:], in0=r_k8[:], scalar=-1.0, in1=r_z[:], op0=ALU.add, op1=ALU.add), ["r_k8", "r_z"], ["r_f8"])
                V(L("tensor_copy", out=slots[:, n, :], in_=r_f8[:]), ["r_f8"], [f"slots{n}"])
                for k in range(8):
                    S.idma(tl, bass.IndirectOffsetOnAxis(ap=slots[:, n, k:k + 1], axis=0), tokc[:, n:n + 1], None, d_sc,
                           reads=[f"slots{n}"], writes=[f"tlw{n}_{k}"])
            def run_first(g):
                for v in g:
                    if v == "HALF":
                        return
            def step(g):
                try:
                    next(g)
                    return True
                except StopIteration:
                    return False
            prev = None
            for n in range(nrun):
                g = chunk(n)
                if prev is None:
                    run_first(g)
                else:
                    first_done = False; second_done = False
                    while not (first_done and second_done):
                        if not first_done:
                            try:
                                v = next(g)
                                if v == "HALF":
                                    first_done = True
                            except StopIteration:
                                first_done = True
                        if not second_done:
                            second_done = not step(prev)
                prev = g
            while step(prev):
                pass
            S.barrier([d_init, d_st_x, d_st_b, d_st_c, d_sc] + d_x + d_p + d_r)
            S.flush()
            if upto == 1:
                return nc

        with ExitStack() as st:
            sb = lambda n, s, d=F32: st.enter_context(nc.sbuf_tensor(n, list(s), d))
            ps = lambda n, s, d=F32: st.enter_context(nc.psum_tensor(n, list(s), d))
            NW = 3
            w13b = [sb(f"w13b{i}", [128, 8, 512], BF16) for i in range(NW)]
            w2b = [sb(f"w2b{i}", [128, 2, D], BF16) for i in range(NW)]
            idx = [sb(f"idx{i}", [128, NJ], I32) for i in range(2)]
            cg = [sb(f"cg{i}", [128, NJ, NE]) for i in range(2)]
            xg = [sb(f"xg{i}", [128, NJ, D], BF16) for i in range(2)]
            xgT = [sb(f"xgT{i}", [128, 8, CAP], BF16) for i in range(2)]
            slh = [sb(f"slh{i}", [128, 512]) for i in range(2)]
            hT = sb("hT", [128, 2, CAP], BF16)
            ys = [sb(f"ys{i}", [128, NJ, D]) for i in range(2)]
            TP = [ps(f"TP{i}", [128, 8, 128], BF16) for i in range(2)]
            HB = [ps(f"HB{i}", [128, 512]) for i in range(4)]
            YB = [ps(f"YB{i}", [128, 512]) for i in range(2)]
            d_wt = [S.dsem(f"wt{i}") for i in range(NW)]
            d_g = [S.dsem(f"g{i}") for i in range(2)]
            d_i = [S.dsem(f"i{i}") for i in range(2)]
            d_y = [S.dsem(f"y{i}") for i in range(2)]

            def load_w(e_):
                b = e_ % NW
                S.dma("pool", w13b[b][:, :, 0:256], w1[e_].rearrange("(k p) n -> p k n", p=128), d_wt[b], writes=[f"wts{b}"])
                S.dma("pool", w13b[b][:, :, 256:512], w3[e_].rearrange("(k p) n -> p k n", p=128), d_wt[b], writes=[f"wts{b}"])
                S.dma("pool", w2b[b][:], w2[e_].rearrange("(k p) n -> p k n", p=128), d_wt[b], writes=[f"wts{b}"])

            def load_g(e_):
                b = e_ % 2
                S.dma("sp", idx[b][:], tl[e_ * CAP:(e_ + 1) * CAP, :].rearrange("(p j) o -> p (j o)", j=NJ), d_i[b],
                      writes=[f"idx{b}"])
                for j in range(NJ):
                    S.idma(xg[b][:, j, :], None, x1d, bass.IndirectOffsetOnAxis(ap=idx[b][:, j:j + 1], axis=0), d_g[b],
                           reads=[f"idx{b}"], writes=[f"gath{b}"])
                    S.idma(cg[b][:, j, :], None, cmatd, bass.IndirectOffsetOnAxis(ap=idx[b][:, j:j + 1], axis=0), d_g[b],
                           reads=[f"idx{b}"], writes=[f"gath{b}"])
                S.readers[f"idx{b}"] = [(d_g[b], S.cnt[d_g[b]])]

            tsl = [(0, 512), (512, CAP - 512)] if CAP > 512 else [(0, CAP)]
            load_w(0); load_g(0); load_w(1)
            for e_ in range(NE):
                b = e_ % 2; wb_ = e_ % NW
                if e_ + 1 < NE:
                    load_g(e_ + 1)
                if e_ + 2 < NE:
                    load_w(e_ + 2)
                for j in range(NJ):
                    tp = j % 2
                    for k in range(8):
                        S.op("pe", L("transpose", out=TP[tp][:, k, :], in_=xg[b][:, j, k * 128:(k + 1) * 128], identity=identb[:]),
                             reads=[f"gath{b}"], writes=[f"TP{tp}"], inc=(k == 7))
                    eng = "act" if j % 2 == 0 else "dve"
                    if eng == "act":
                        S.op("act", L("copy", out=xgT[b][:, :, j * 128:(j + 1) * 128], in_=TP[tp][:]), reads=[f"TP{tp}"], writes=[f"xgT{b}"])
                    else:
                        S.op("dve", L("tensor_copy", out=xgT[b][:, :, j * 128:(j + 1) * 128], in_=TP[tp][:]), reads=[f"TP{tp}"], writes=[f"xgT{b}"])
                for ti, (t0, tn) in enumerate(tsl):
                    for m in range(4):
                        for k in range(8):
                            S.op("pe", L("matmul", HB[m][:, 0:tn], lhsT=w13b[wb_][:, k, m * 128:(m + 1) * 128], rhs=xgT[b][:, k, t0:t0 + tn],
                                                           start=(k == 0), stop=(k == 7)),
                                 reads=[f"wts{wb_}", f"xgT{b}"], writes=[f"HB{m}"], inc=(k == 7))
                    for m in range(2):
                        S.op("act", L("activation", out=slh[m][:, 0:tn], in_=HB[m][:, 0:tn], func=AF.Silu), reads=[f"HB{m}"], writes=[f"slh{m}"])
                        S.op("dve", L("tensor_tensor", out=hT[:, m, t0:t0 + tn], in0=slh[m][:, 0:tn], in1=HB[2 + m][:, 0:tn], op=ALU.mult),
                             reads=[f"slh{m}", f"HB{2 + m}"], writes=["hT"])
                for j in range(NJ):
                    for hf in range(2):
                        for c in range(2):
                            S.op("pe", L("matmul", YB[hf][:], lhsT=hT[:, c, j * 128:(j + 1) * 128], rhs=w2b[wb_][:, c, hf * 512:(hf + 1) * 512],
                                                           start=(c == 0), stop=(c == 1)),
                                 reads=["hT", f"wts{wb_}"], writes=[f"YB{hf}"], inc=(c == 1))
                    S.op("act", L("activation", out=ys[b][:, j, 0:512], in_=YB[0][:], func=AF.Copy, scale=cg[b][:, j, e_:e_ + 1]),
                         reads=["YB0", f"gath{b}"], writes=[f"ys{b}"])
                    S.op("dve", L("tensor_scalar", out=ys[b][:, j, 512:1024], in0=YB[1][:], scalar1=cg[b][:, j, e_:e_ + 1], scalar2=None, op0=ALU.mult),
                         reads=["YB1", f"gath{b}"], writes=[f"ys{b}"])
                S.dma("sp", Yd[e_ * CAP:(e_ + 1) * CAP, :].rearrange("(p j) d -> p j d", j=NJ), ys[b][:], d_y[b],
                      reads=[f"ys{b}"], writes=[f"Yd{e_}"])
            S.barrier(d_y)
            S.flush()
            if upto == 2:
                return nc

        with ExitStack() as st:
            sb = lambda n, s, d=F32: st.enter_context(nc.sbuf_tensor(n, list(s), d))
            bt = [sb(f"bt{i}", [128, D]) for i in range(2)]
            yk = [sb(f"yk{i}", [128, 8, D]) for i in range(2)]
            acc = [sb(f"acc{i}", [128, D]) for i in range(2)]
            accp = [sb(f"accp{i}", [128, D]) for i in range(2)]
            bn2 = sb("bn2", [128, 2, 6]); mv2 = sb("mv2", [128, 2]); rs2 = sb("rs2", [128, 1]); nh2 = sb("nh2", [128, 1]); nm2 = sb("nm2", [128, 1])
            d_b = [S.dsem(f"b{i}") for i in range(2)]
            d_k = [S.dsem(f"k{i}") for i in range(2)]
            d_o = [S.dsem(f"o{i}") for i in range(2)]
            S.op("dve", L("memset", nh2[:], -0.5), writes=["nh2"])

            def load3(n):
                i = n % 2
                S.dma("sp", bt[i][:], based[n * 128:(n + 1) * 128, :], d_b[i], writes=[f"bt{i}"])
                for k in range(8):
                    S.idma(yk[i][:, k, :], None, Yd, bass.IndirectOffsetOnAxis(ap=slots[:, n, k:k + 1], axis=0), d_k[i],
                           writes=[f"yk{i}"])

            load3(0)
            for n in range(NCH):
                i = n % 2
                if n + 1 < NCH:
                    load3(n + 1)
                S.op("pool", L("tensor_tensor", out=accp[i][:], in0=yk[i][:, 5, :], in1=yk[i][:, 6, :], op=ALU.add), reads=[f"yk{i}"], writes=[f"accp{i}"])
                S.op("pool", L("tensor_tensor", out=accp[i][:], in0=accp[i][:], in1=yk[i][:, 7, :], op=ALU.add), reads=[f"yk{i}", f"accp{i}"], writes=[f"accp{i}"])
                S.op("dve", L("tensor_tensor", out=acc[i][:], in0=yk[i][:, 0, :], in1=yk[i][:, 1, :], op=ALU.add), reads=[f"yk{i}"], writes=[f"acc{i}"])
                for k in (2, 3, 4):
                    S.op("dve", L("tensor_tensor", out=acc[i][:], in0=acc[i][:], in1=yk[i][:, k, :], op=ALU.add), reads=[f"yk{i}", f"acc{i}"], writes=[f"acc{i}"])
                S.op("dve", L("tensor_tensor", out=acc[i][:], in0=acc[i][:], in1=bt[i][:], op=ALU.add), reads=[f"bt{i}", f"acc{i}"], writes=[f"acc{i}"])
                S.op("dve", L("tensor_tensor", out=acc[i][:], in0=acc[i][:], in1=accp[i][:], op=ALU.add), reads=[f"accp{i}", f"acc{i}"], writes=[f"acc{i}"])
                for hf in range(2):
                    S.op("dve", L("bn_stats", out=bn2[:, hf, :], in_=acc[i][:, hf * 512:(hf + 1) * 512]), reads=[f"acc{i}"], writes=["bn2"])
                S.op("dve", L("bn_aggr", out=mv2[:], in_=bn2[:].rearrange("p a b -> p (a b)")), reads=["bn2"], writes=["mv2"])
                S.op("dve", L("tensor_scalar", out=rs2[:], in0=mv2[:, 1:2], scalar1=1e-5, scalar2=None, op0=ALU.add), reads=["mv2"], writes=["rs2"])
                S.op("act", L("activation", out=rs2[:], in_=rs2[:], func=AF.Ln), reads=["rs2"], writes=["rs2"])
                S.op("act", L("activation", out=rs2[:], in_=rs2[:], func=AF.Exp, scale=-0.5), reads=["rs2"], writes=["rs2"])
                S.op("dve", L("scalar_tensor_tensor", out=nm2[:], in0=mv2[:, 0:1], scalar=-1.0, in1=rs2[:], op0=ALU.mult, op1=ALU.mult),
                     reads=["mv2", "rs2"], writes=["nm2"])
                S.op("act", L("activation", out=acc[i][:], in_=acc[i][:], func=AF.Identity, bias=nm2[:, 0:1], scale=rs2[:, 0:1]),
                     reads=[f"acc{i}", "nm2", "rs2"], writes=[f"acc{i}"])
                S.op("dve", L("tensor_tensor", out=acc[i][:], in0=acc[i][:], in1=ln2g[:], op=ALU.mult), reads=[f"acc{i}", "ln2g"], writes=[f"acc{i}"])
                S.op("dve", L("tensor_tensor", out=acc[i][:], in0=acc[i][:], in1=ln2b[:], op=ALU.add), reads=[f"acc{i}", "ln2b"], writes=[f"acc{i}"])
                S.dma("sp", out[n * 128:(n + 1) * 128, :], acc[i][:], d_o[i], reads=[f"acc{i}"], writes=[f"out{n}"])
            S.wait_all("sp", [f"out{n}" for n in range(NCH)])
            S.wait_all("pool", [f"out{n}" for n in range(NCH)])
            S.flush()
    return nc


def _consts():
    h = np.arange(8, dtype=np.float64)
    gam = 1.0 - 2.0 ** (-5.0 - h)
    lg = np.log(gam)
    idx = np.arange(128, dtype=np.float64)
    pos = np.arange(S_LEN, dtype=np.float32)
    def tabs(half, theta):
        inv = (np.float32(theta) ** (-(np.arange(half, dtype=np.float32) / np.float32(half)))).astype(np.float32)
        ang = (pos[:, None] * inv[None, :]).astype(np.float32)
        c, s = np.cos(ang).astype(np.float32), np.sin(ang).astype(np.float32)
        return np.concatenate([c, c], 1), np.concatenate([-s, s], 1)
    ccr, ssr = tabs(32, 10000.0)
    cca, ssa = tabs(8, 500000.0)
    rope = np.concatenate([ccr, ssr, cca, ssa], 1).reshape(NCH, 128, 160).astype(np.float32)
    diff = idx[None, :] - idx[:, None]
    dt = np.where(diff[:, None, :] >= 0, np.exp(np.maximum(diff, 0)[:, None, :] * lg[None, :, None]), 0.0) / 8.0
    qw = np.zeros((128, 4, 128)); cd = np.zeros((128, 4, 64))
    for hp in range(2):
        for j in range(4):
            hh = 2 * j + hp
            qw[hp * 64:(hp + 1) * 64, j, :] = np.exp((idx + 1) * lg[hh])[None, :]
            cd[hp * 64:(hp + 1) * 64, j, :] = np.exp(128 * lg[hh])
    kwt = np.exp((127 - idx)[:, None] * lg[None, :]) / 8.0
    kk = idx[:, None]; qq = idx[None, :]
    mcur = (kk <= qq).astype(np.float32)
    mprev = (kk > qq).astype(np.float32)
    utri = (kk < qq).astype(np.float32)
    eoff = np.tile((np.arange(NE, dtype=np.float32) * CAP + 1.0)[None, :], (128, 1))
    tok = (np.arange(NCH)[None, :] * 128 + np.arange(128)[:, None]).astype(np.int32)
    f = lambda a: np.ascontiguousarray(a, dtype=np.float32)
    return {"c_ident": f(np.eye(128)), "c_rope": f(rope), "c_dt": f(dt.reshape(128, 1024)), "c_qw": f(qw.reshape(128, 512)),
            "c_kw": f(kwt), "c_cd": f(cd.reshape(128, 256)), "c_mcur": f(mcur), "c_mprev": f(mprev), "c_utri": f(utri),
            "c_ones": f(np.ones((128, 128))), "c_eoff": f(eoff), "c_tok": np.ascontiguousarray(tok)}


def _rep(v, n=128):
    return np.ascontiguousarray(np.broadcast_to(np.asarray(v, dtype=np.float32).reshape(1, -1), (n, v.size)))


def make_in_maps(x, p, w_in, ret_gn_gain, attn_scale, sinks, w_out, ln1_g, ln1_b, w_router, router_bias,
                 w1, w3, w2, ws1, ws3, ws2, w_ple_gate, b_ple_gate, w_ple_proj, ln2_g, ln2_b):
    c = _consts()
    shared = {
        "w_in": np.ascontiguousarray(w_in[0]), "w_out": np.ascontiguousarray(w_out[0]), "w_router": np.ascontiguousarray(w_router[0]),
        "w1": np.ascontiguousarray(w1[0]), "w3": np.ascontiguousarray(w3[0]), "w2": np.ascontiguousarray(w2[0]),
        "ws1": np.ascontiguousarray(ws1[0]), "ws3": np.ascontiguousarray(ws3[0]), "ws2": np.ascontiguousarray(ws2[0]),
        "w_ple_gate": np.ascontiguousarray(w_ple_gate[0]), "w_ple_proj": np.ascontiguousarray(w_ple_proj[0]),
        "gain_r": _rep(ret_gn_gain[0]), "ascale_r": _rep(attn_scale[0]), "sinks_r": _rep(sinks[0]),
        "ln1g_r": _rep(ln1_g[0]), "ln1b_r": _rep(ln1_b[0]), "bgate_r": _rep(b_ple_gate[0]),
        "ln2g_r": _rep(ln2_g[0]), "ln2b_r": _rep(ln2_b[0]), "rbias_r": _rep(router_bias[0]),
    }
    shared.update(c)
    maps = []
    for b in range(8):
        m = dict(shared)
        m["x"] = np.ascontiguousarray(x[b]); m["p"] = np.ascontiguousarray(p[0, b])
        maps.append(m)
    return maps


def kernel(**inputs):
    inputs = {k: np.asarray(v) for k, v in inputs.items()}
    nc = build()
    maps = make_in_maps(**inputs)
    res = run_bass_kernel_spmd(nc, maps, core_ids=list(range(8)))
    return np.stack([np.asarray(r["out"], dtype=np.float32) for r in res.results], axis=0)
```

```python
import os
import numpy as np
from contextlib import ExitStack
import concourse.bass as bass
import concourse.mybir as mybir
from concourse.bass_utils import run_bass_kernel_spmd

F32 = mybir.dt.float32
BF16 = mybir.dt.bfloat16
I32 = mybir.dt.int32
AF = mybir.ActivationFunctionType
ALU = mybir.AluOpType
AX = mybir.AxisListType

S_LEN = 4096
D = 1024
NCH = S_LEN // 128
NE = 64
CAP = 768
NJ = CAP // 128
ALPHA = 2.0 ** 0.25
ROPE_ADD_ENG = "dve"
SKIP_AK_ROPE = False
DUMMY_TOK = S_LEN
DUMMY_SLOT = NE * CAP


class Sync:
    def __init__(self, nc, stack):
        self.nc = nc
        self.stack = stack
        self.eng = {"pe": nc.tensor, "act": nc.scalar, "dve": nc.vector, "pool": nc.gpsimd, "sp": nc.sync}
        self.sems = {}
        self.cnt = {}
        for e in ("pe", "act", "dve", "pool"):
            self.sems[e] = stack.enter_context(nc.semaphore("s_" + e))
            self.cnt[e] = 0
        self.waited = {e: {} for e in self.eng}
        self.last_w = {}
        self.readers = {}
        self.q = {e: [] for e in self.eng}
        self.sp_recent = []
        self.excl = set(["TB0", "TB1a", "TB1b", "F0", "F1", "F2", "F3", "F4", "TP0", "TP1", "HB0", "HB1", "HB2", "HB3", "YB0", "YB1"])

    def dsem(self, name):
        s = self.stack.enter_context(self.nc.semaphore("d_" + name))
        self.sems["d_" + name] = s
        self.cnt["d_" + name] = 0
        return "d_" + name

    def _need(self, e, key, val):
        if self.waited[e].get(key, 0) >= val:
            return
        self.waited[e][key] = val
        self.q[e].append(("wait", self.sems[key], val))

    def _deps(self, e, reads, writes, skip=None):
        for r in reads:
            w = self.last_w.get(r)
            if w is not None and not (w[0] == e == "pe"):
                self._need(e, w[0], w[1])
            if r in self.excl:
                for (k, v) in self.readers.get(r, []):
                    if k != e:
                        self._need(e, k, v)
        for wkey in writes:
            w = self.last_w.get(wkey)
            if w is not None and not (w[0] == e == "pe") and w[0] != skip:
                self._need(e, w[0], w[1])
            for (k, v) in self.readers.get(wkey, []):
                if not (k == e == "pe") and k != skip:
                    self._need(e, k, v)

    def barrier(self, dsems=()):
        for e in ("pe", "act", "dve", "pool", "sp"):
            for k in ("pe", "act", "dve", "pool"):
                if k != e and self.cnt[k] > 0:
                    self._need(e, k, self.cnt[k])
            for k in dsems:
                if self.cnt[k] > 0:
                    self._need(e, k, self.cnt[k])

    def _commit(self, key, val, reads, writes):
        for r in reads:
            self.readers.setdefault(r, []).append((key, val))
        for w in writes:
            self.last_w[w] = (key, val)
            self.readers[w] = []

    def op(self, e, fn, reads=(), writes=(), inc=True):
        self._deps(e, reads, writes)
        val = self.cnt[e] + 1
        self._commit(e, val, reads, writes)
        self.q[e].append(("ins", fn, self.sems[e] if inc else None, 1))
        if inc:
            self.cnt[e] = val

    def dma(self, q, out, in_, dsem, reads=(), writes=(), **kw):
        self._deps(q, reads, writes, skip=dsem)
        self.q[q].append(("ins", L("dma_start", out=out, in_=in_, **kw), self.sems[dsem], 16))
        self.cnt[dsem] += 16
        self._commit(dsem, self.cnt[dsem], reads, writes)

    def idma(self, out, out_off, in_, in_off, dsem, reads=(), writes=(), **kw):
        self._deps("pool", reads, writes, skip=dsem)
        self.q["pool"].append(("ins", L("indirect_dma_start", out=out, out_offset=out_off, in_=in_, in_offset=in_off, **kw), self.sems[dsem], 16))
        self.cnt[dsem] += 16
        self._commit(dsem, self.cnt[dsem], reads, writes)

    def flush(self):
        def replay(items):
            def run(eng):
                for it in items:
                    if it[0] == "wait":
                        eng.wait_ge(it[1], it[2])
                    else:
                        ins = it[1](eng)
                        if it[2] is not None:
                            ins.then_inc(it[2], it[3])
            return run
        qs = self.q
        self.q = {e: [] for e in self.eng}
        with self.nc.Block() as block:
            block.tensor(replay(qs["pe"]))
            block.scalar(replay(qs["act"]))
            block.vector(replay(qs["dve"]))
            block.gpsimd(replay(qs["pool"]))
            block.sync(replay(qs["sp"]))

    def wait_all(self, e, keys):
        for k in keys:
            w = self.last_w.get(k)
            if w is not None:
                self._need(e, w[0], w[1])


def L(name, *a, **k):
    return lambda e: getattr(e, name)(*a, **k)


def bc(ap, shape, axis):
    return ap.unsqueeze(axis).to_broadcast(list(shape))


def build(debug=False, upto=3, nrun=NCH, stage=99):
    nc = bass.Bass("TRN2", target_bir_lowering=False)

    def din(name, shape, dt=F32):
        return nc.dram_tensor(name, list(shape), dt, kind="ExternalInput").ap()

    x = din("x", [S_LEN, D]); p_in = din("p", [S_LEN, 256])
    w_in = din("w_in", [D, 2816]); w_out = din("w_out", [D, D]); w_router = din("w_router", [D, NE])
    NEd = 1 if upto == 1 else NE
    w1 = din("w1", [NEd, D, 256]); w3 = din("w3", [NEd, D, 256]); w2 = din("w2", [NEd, 256, D])
    ws1 = din("ws1", [D, 256]); ws3 = din("ws3", [D, 256]); ws2 = din("ws2", [256, D])
    w_pg = din("w_ple_gate", [D, D]); w_pp = din("w_ple_proj", [256, D])
    gain_r = din("gain_r", [128, 512]); ascale_r = din("ascale_r", [128, 512]); sinks_r = din("sinks_r", [128, 8])
    ln1g_r = din("ln1g_r", [128, D]); ln1b_r = din("ln1b_r", [128, D]); bgate_r = din("bgate_r", [128, D])
    ln2g_r = din("ln2g_r", [128, D]); ln2b_r = din("ln2b_r", [128, D]); rbias_r = din("rbias_r", [128, NE])
    c_ident = din("c_ident", [128, 128]); c_rope = din("c_rope", [NCH, 128, 160]); c_dt = din("c_dt", [128, 1024])
    c_qw = din("c_qw", [128, 512]); c_kw = din("c_kw", [128, 8]); c_cd = din("c_cd", [128, 256])
    c_mcur = din("c_mcur", [128, 128]); c_mprev = din("c_mprev", [128, 128]); c_utri = din("c_utri", [128, 128])
    c_ones = din("c_ones", [128, 128]); c_eoff = din("c_eoff", [128, NE]); c_tok = din("c_tok", [128, NCH], I32)
    okind = "ExternalOutput" if debug else "Internal"
    out = nc.dram_tensor("out", [S_LEN, D], F32, kind="ExternalOutput").ap()
    x1d = nc.dram_tensor("x1d", [S_LEN + 1, D], BF16, kind=okind).ap()
    based = nc.dram_tensor("based", [S_LEN, D], F32, kind=okind).ap()
    cmatd = nc.dram_tensor("cmatd", [S_LEN + 1, NE], F32, kind=okind).ap()
    tl = nc.dram_tensor("tl", [NE * CAP + 128, 1], I32, kind=okind).ap()
    Yd = nc.dram_tensor("Yd", [NE * CAP + 1, D], F32, kind=okind).ap()

    with ExitStack() as st0:
        S = Sync(nc, st0)
        sb0 = lambda n, s, d=F32: st0.enter_context(nc.sbuf_tensor(n, list(s), d))
        slots = sb0("slots", [128, NCH, 8], I32)
        ln2g = sb0("ln2g", [128, D]); ln2b = sb0("ln2b", [128, D])
        identb = sb0("identb", [128, 128], BF16)
        d_misc = S.dsem("misc")
        d_init = S.dsem("init")
        S.dma("sp", ln2g[:], ln2g_r, d_misc, writes=["ln2g"])
        S.dma("sp", ln2b[:], ln2b_r, d_misc, writes=["ln2b"])
        d_miscp = S.dsem("miscp")
        S.dma("pool", identb[:], c_ident, d_miscp, writes=["identb"])

        with ExitStack() as st:
            sb = lambda n, s, d=F32: st.enter_context(nc.sbuf_tensor(n, list(s), d))
            ps = lambda n, s, d=F32: st.enter_context(nc.psum_tensor(n, list(s), d))
            winb = sb("winb", [128, 8, 2816], BF16); woutb = sb("woutb", [128, 8, D], BF16)
            wr = sb("wr", [128, 8, NE]); wsb = sb("wsb", [128, 8, 512], BF16); ws2b = sb("ws2b", [128, 2, D], BF16)
            wgb = sb("wgb", [128, 8, D], BF16); wpb = sb("wpb", [128, 2, D], BF16)
            d_w = S.dsem("w")
            for k in range(8):
                S.dma("pool", winb[:, k, :], w_in[k * 128:(k + 1) * 128, :], d_w, writes=["winb"])
            S.dma("pool", woutb[:], w_out.rearrange("(k p) n -> p k n", p=128), d_w, writes=["woutb"])
            S.dma("sp", wr[:], w_router.rearrange("(k p) n -> p k n", p=128), d_misc, writes=["wr"])
            S.dma("pool", wsb[:, :, 0:256], ws1.rearrange("(k p) n -> p k n", p=128), d_w, writes=["wsb"])
            S.dma("pool", wsb[:, :, 256:512], ws3.rearrange("(k p) n -> p k n", p=128), d_w, writes=["wsb"])
            S.dma("pool", ws2b[:], ws2.rearrange("(k p) n -> p k n", p=128), d_w, writes=["ws2b"])
            S.dma("pool", wgb[:], w_pg.rearrange("(k p) n -> p k n", p=128), d_w, writes=["wgb"])
            S.dma("pool", wpb[:], w_pp.rearrange("(k p) n -> p k n", p=128), d_w, writes=["wpb"])
            identf = sb("identf", [128, 128]); dtm = sb("dtm", [128, 8, 128]); qw = sb("qw", [128, 4, 128])
            kwt = sb("kwt", [128, 8]); cdt = sb("cdt", [128, 4, 64])
            mcur = sb("mcur", [128, 128], BF16); mprev = sb("mprev", [128, 128], BF16)
            utri = sb("utri", [128, 128], BF16); onesb = sb("onesb", [128, 128], BF16)
            eoff = sb("eoff", [128, NE]); tokc = sb("tokc", [128, NCH], I32)
            gain = sb("gain", [128, 512]); ascale = sb("ascale", [128, 512]); esink = sb("esink", [128, 8])
            ln1g = sb("ln1g", [128, D]); ln1b = sb("ln1b", [128, D]); bgb = sb("bgb", [1, D], BF16)
            rbias = sb("rbias", [128, NE]); neghalf = sb("neghalf", [128, 8])
            d_c = S.dsem("c"); d_cp = S.dsem("cp")
            for (t, src, q) in ((identf, c_ident, "sp"), (dtm, c_dt.rearrange("p (h q) -> p h q", q=128), "sp"),
                                (qw, c_qw.rearrange("p (j q) -> p j q", q=128), "sp"), (kwt, c_kw, "sp"),
                                (cdt, c_cd.rearrange("p (j e) -> p j e", e=64), "sp"),
                                (mcur, c_mcur, "pool"), (mprev, c_mprev, "pool"), (utri, c_utri, "pool"),
                                (onesb, c_ones, "pool"), (eoff, c_eoff, "sp"), (tokc, c_tok, "sp"),
                                (gain, gain_r, "sp"), (ascale, ascale_r, "sp"), (esink, sinks_r, "sp"),
                                (ln1g, ln1g_r, "sp"), (ln1b, ln1b_r, "sp"), (bgb, bgate_r[0:1, :], "pool"),
                                (rbias, rbias_r, "sp")):
                S.dma(q, t[:], src, d_c if q == "sp" else d_cp)
            for e in ("pe", "act", "dve", "pool"):
                for dk in (d_c, d_cp, d_w, d_misc, d_miscp):
                    S._need(e, dk, S.cnt[dk])
            S.op("act", L("activation", out=esink[:], in_=esink[:], func=AF.Exp), writes=["esink"])
            S.op("dve", L("memset", neghalf[:], -0.5), writes=["neghalf"])

            xf = [sb(f"xf{i}", [128, D]) for i in range(2)]
            pf = [sb(f"pf{i}", [128, 256]) for i in range(2)]
            rope = [sb(f"rope{i}", [128, 160]) for i in range(2)]
            xb = sb("xb", [128, D], BF16); xT = sb("xT", [128, 8, 128], BF16)
            pb = sb("pb", [128, 256], BF16); ppT_ = [sb(f"ppT{i_}", [128, 2, 128], BF16) for i_ in range(2)]
            ropA = sb("ropA", [128, 8, 64]); ropB = sb("ropB", [128, 8, 64])
            rq_rot = sb("rq_rot", [128, 8, 64], BF16); rk_rot = sb("rk_rot", [128, 8, 64], BF16)
            kw = sb("kw", [128, 8, 64], BF16); rv = sb("rv", [128, 512], BF16); sg = sb("sg", [128, 512])
            aq_st = sb("aq_st", [128, 8, 64], BF16); ak_rot = sb("ak_rot", [128, 2, 64], BF16); akvf = sb("akvf", [128, 256])
            vaug = [sb(f"vaug{i}", [128, 2, 66], BF16) for i in range(2)]
            akT = [sb(f"akT{i}", [128, 128], BF16) for i in range(2)]
            rqkT = sb("rqkT", [128, 8, 128], BF16); rqTw = sb("rqTw", [128, 4, 128], BF16)
            aqT = sb("aqT", [128, 4, 128], BF16)
            sd = sb("sd", [128, 8, 128], BF16)
            state = sb("state", [128, 4, 64]); sttmp = sb("sttmp", [128, 4, 64]); stateb = sb("stateb", [128, 4, 64], BF16)
            gs = sb("gs", [128, 512])
            st1 = sb("st1", [128, 8]); st2 = sb("st2", [128, 8]); stm = sb("stm", [128, 8]); stv = sb("stv", [128, 8])
            strs = sb("strs", [128, 8])
            eT = [sb(f"eT{i}", [128, 2, 512], BF16) for i in range(2)]
            den = sb("den", [128, 8]); rden = sb("rden", [128, 8]); att_t = sb("att_t", [128, 8, 64])
            mixcat_ = [sb(f"mixcat{i_}", [128, D], BF16) for i_ in range(2)]; mixT = sb("mixT", [128, 8, 128], BF16)
            bigA = sb("bigA", [128, D]); x1 = sb("x1", [128, D]); bigC = sb("bigC", [128, D])
            bnst = sb("bnst", [128, 2, 6]); mv = sb("mv", [128, 2]); rstd = sb("rstd", [128, 1]); nmr = sb("nmr", [128, 1])
            x1Tf = sb("x1Tf", [128, 8, 128]); x1Tb = sb("x1Tb", [128, 8, 128], BF16)
            sl = sb("sl", [128, 2, 128]); hsT = sb("hsT", [128, 2, 128], BF16)
            scores = sb("scores", [128, NE]); biased = sb("biased", [128, NE]); r_eq = sb("r_eq", [128, NE])
            r_b2 = sb("r_b2", [128, NE]); r_m1 = sb("r_m1", [128, 8]); r_m2 = sb("r_m2", [128, 8]); r_gs = sb("r_gs", [128, 8])
            r_gtop = sb("r_gtop", [128, 8]); r_gm = sb("r_gm", [128, 8]); r_neg = sb("r_neg", [128, 8])
            r_msk = sb("r_msk", [128, NE]); r_top = sb("r_top", [128, 8]); r_M = sb("r_M", [128, NE]); r_Mb = sb("r_Mb", [128, NE], BF16)
            r_w = sb("r_w", [128, NE]); r_ws = sb("r_ws", [128, 1]); r_rs = sb("r_rs", [128, 1]); cm = sb("cm", [128, NE])
            r_pos = sb("r_pos", [128, NE]); r_run = sb("r_run", [128, NE]); r_key = sb("r_key", [128, NE]); r_val = sb("r_val", [128, NE])
            r_k8 = sb("r_k8", [128, 8]); r_z = sb("r_z", [128, 8]); r_f8 = sb("r_f8", [128, 8])
            TB0 = ps("TB0", [128, 8, 128], BF16); TB1 = ps("TB1", [128, 16, 128], BF16)
            Fb = [ps(f"F{i}", [128, 512]) for i in range(5)]

            tli = sb("tli", [128, NE * CAP // 128], I32)
            S.op("dve", L("memset", tli[:], DUMMY_TOK), writes=["tli"])
            S.op("dve", L("memset", bigC[:], 0.0), writes=["bigC"])
            S.op("dve", L("memset", xb[:], 0.0), writes=["xb"])
            S.dma("sp", tl[0:NE * CAP, :].rearrange("(p f) o -> p (f o)", p=128), tli[:], d_init, reads=["tli"], writes=["tl"])
            S.dma("sp", tl[NE * CAP:NE * CAP + 128, :], tli[:, 0:1], d_init, reads=["tli"], writes=["tl"])
            S.dma("sp", x1d[S_LEN:S_LEN + 1, :], xb[0:1, :], d_init, reads=["xb"], writes=["x1d_z"])
            S.dma("sp", cmatd[S_LEN:S_LEN + 1, :], bigC[0:1, 0:NE], d_init, reads=["bigC"], writes=["cmatd_z"])
            S.dma("sp", Yd[DUMMY_SLOT:DUMMY_SLOT + 1, :], bigC[0:1, :], d_init, reads=["bigC"], writes=["Yd_z"])
            for e in ("pe", "act", "dve", "pool", "sp"):
                S._need(e, d_init, S.cnt[d_init])

            S.op("dve", L("memset", state[:], 0.0), writes=["state"])
            S.op("dve", L("memset", stateb[:], 0.0), writes=["stateb"])
            S.op("dve", L("memset", r_run[:], 0.0), writes=["r_run"])
            for i in range(2):
                S.op("dve", L("memset", vaug[i][:], 1.0), writes=[f"vaug{i}"])

            d_x = [S.dsem(f"x{i}") for i in range(2)]
            d_p = [S.dsem(f"p{i}") for i in range(2)]
            d_r = [S.dsem(f"r{i}") for i in range(2)]
            d_st_x = S.dsem("stx"); d_st_b = S.dsem("stb"); d_st_c = S.dsem("stc")
            d_sc = S.dsem("sc")

            def load_chunk(n):
                i = n % 2
                S.dma("sp", xf[i][:], x[n * 128:(n + 1) * 128, :], d_x[i], writes=[f"xf{i}"])
                S.dma("sp", pf[i][:], p_in[n * 128:(n + 1) * 128, :], d_p[i], writes=[f"pf{i}"])
                S.dma("sp", rope[i][:], c_rope[n], d_r[i], writes=[f"rope{i}"])

            def rope_apply(src, H, half, cc, ss, dst, key_src, key_dst, rkey, Hout=None):
                w2_ = 2 * half
                nops = int(os.environ.get("K_ROPE_N", "4")) if half == 8 else 4
                if nops >= 1:
                    S.op("dve", L("tensor_tensor", out=ropA[:, 0:H, 0:w2_], in0=src[:, :, 0:w2_],
                                  in1=bc(cc, [128, H, w2_], 1), op=ALU.mult),
                         reads=[key_src, rkey], writes=["ropA"])
                if nops >= 2:
                    S.op("dve", L("tensor_tensor", out=ropB[:, 0:H, 0:half], in0=src[:, :, half:w2_],
                                  in1=bc(ss[:, 0:half], [128, H, half], 1), op=ALU.mult),
                         reads=[key_src, rkey], writes=["ropB"])
                if nops >= 3:
                    S.op("dve", L("tensor_tensor", out=ropB[:, 0:H, half:w2_], in0=src[:, :, 0:half],
                                  in1=bc(ss[:, half:w2_], [128, H, half], 1), op=ALU.mult),
                         reads=[key_src, rkey], writes=["ropB"])
                Ho = Hout or H
                if nops >= 4:
                    S.op(ROPE_ADD_ENG, L("tensor_tensor", out=dst, in0=ropA[:, 0:Ho, 0:w2_], in1=ropB[:, 0:Ho, 0:w2_], op=ALU.add),
                         reads=["ropA", "ropB"], writes=[key_dst])

            load_chunk(0)
            def chunk(n):
                i = n % 2
                rk_ = f"rope{i}"
                rp = rope[i]
                S.op("act", L("copy", out=xb[:], in_=xf[i][:]), reads=[f"xf{i}"], writes=["xb"])
                S.op("act", L("copy", out=pb[:], in_=pf[i][:]), reads=[f"pf{i}"], writes=["pb"])
                for k in range(8):
                    S.op("pe", L("transpose", out=TB0[:, k, :], in_=xb[:, k * 128:(k + 1) * 128], identity=identb[:]),
                         reads=["xb"], writes=["TB0"], inc=(k == 7))
                S.op("act", L("copy", out=xT[:], in_=TB0[:]), reads=["TB0"], writes=["xT"])
                yield None
                def proj(g, bank):
                    c0 = g * 512
                    wdt = min(512, 2816 - c0)
                    for k in range(8):
                        S.op("pe", L("matmul", Fb[bank][:, 0:wdt], lhsT=xT[:, k, :], rhs=winb[:, k, c0:c0 + wdt],
                                                       start=(k == 0), stop=(k == 7)),
                             reads=["xT"], writes=[f"F{bank}"], inc=(k == 7))
                proj(0, 0); proj(1, 1); proj(2, 2)
                rope_apply(Fb[0][:].rearrange("p (h d) -> p h d", d=64), 8, 32, rp[:, 0:64], rp[:, 64:128],
                           rq_rot[:], "F0", "rq_rot", rk_)
                proj(3, 0)
                rope_apply(Fb[1][:].rearrange("p (h d) -> p h d", d=64), 8, 32, rp[:, 0:64], rp[:, 64:128],
                           rk_rot[:], "F1", "rk_rot", rk_)
                S.op("dve", L("tensor_tensor", out=kw[:], in0=rk_rot[:], in1=bc(kwt[:, :], [128, 8, 64], 2), op=ALU.mult),
                     reads=["rk_rot"], writes=["kw"])
                proj(4, 1)
                S.op("act", L("copy", out=rv[:], in_=Fb[2][:]), reads=["F2"], writes=["rv"])
                proj(5, 2)
                S.op("act", L("activation", out=sg[:], in_=Fb[0][:], func=AF.Silu), reads=["F0"], writes=["sg"])
                aq_src = Fb[1][:].rearrange("p (k g d) -> p k g d", k=2, g=4)
                aq_dst = aq_st[:].rearrange("p (g k) d -> p k g d", k=2)
                S.op("act", L("copy", out=aq_dst, in_=aq_src), reads=["F1"], writes=["aq_st"])
                for hk in range(2):
                    rope_apply(aq_src[:, hk], 4, 8, rp[:, 128:144], rp[:, 144:160],
                               aq_dst[:, hk, :, 0:16], "F1", "aq_st", rk_)
                ak_src = Fb[2][:, 0:128].rearrange("p (h d) -> p h d", d=64)
                S.op("act", L("copy", out=ak_rot[:], in_=ak_src), reads=["F2"], writes=["ak_rot"])
                S.op("act", L("copy", out=akvf[:], in_=Fb[2][:, 0:256]), reads=["F2"], writes=["akvf"])
                if SKIP_AK_ROPE:
                    pass
                else:
                    rope_apply(akvf[:].rearrange("p (h d) -> p h d", d=64), 4, 8, rp[:, 128:144], rp[:, 144:160], ak_rot[:, :, 0:16], "akvf", "ak_rot", rk_, Hout=2)
                S.op("act", L("copy", out=vaug[i][:, :, 0:64], in_=Fb[2][:, 128:256].rearrange("p (h d) -> p h d", d=64)),
                     reads=["F2"], writes=[f"vaug{i}"])
                yield None
                if n + 1 < nrun:
                    load_chunk(n + 1)
                for j in range(4):
                    S.op("pe", L("transpose", out=TB1[:, j, :], in_=rq_rot[:].rearrange("p h d -> p (h d)")[:, j * 128:(j + 1) * 128],
                                                      identity=identb[:]), reads=["rq_rot"], writes=["TB1a"], inc=False)
                for j in range(4):
                    S.op("pe", L("transpose", out=TB1[:, 4 + j, :], in_=rk_rot[:].rearrange("p h d -> p (h d)")[:, j * 128:(j + 1) * 128],
                                                      identity=identb[:]), reads=["rk_rot"], writes=["TB1a"], inc=(j == 3))
                S.op("act", L("copy", out=rqkT[:], in_=TB1[:, 0:8, :]), reads=["TB1a"], writes=["rqkT"])
                S.op("dve", L("tensor_tensor", out=rqTw[:], in0=TB1[:, 0:4, :], in1=qw[:], op=ALU.mult),
                     reads=["TB1a"], writes=["rqTw"])
                for j in range(4):
                    S.op("pe", L("transpose", out=TB1[:, 8 + j, :], in_=aq_st[:].rearrange("p h d -> p (h d)")[:, j * 128:(j + 1) * 128],
                                                      identity=identb[:]), reads=["aq_st"], writes=["TB1b"], inc=False)
                S.op("pe", L("transpose", out=TB1[:, 12, :], in_=ak_rot[:].rearrange("p h d -> p (h d)"), identity=identb[:]),
                     reads=["ak_rot"], writes=["TB1b"], inc=False)
                for c in range(2):
                    S.op("pe", L("transpose", out=TB1[:, 13 + c, :], in_=pb[:, c * 128:(c + 1) * 128], identity=identb[:]),
                         reads=["pb"], writes=["TB1b"], inc=(c == 1))
                S.op("act", L("copy", out=aqT[:], in_=TB1[:, 8:12, :]), reads=["TB1b"], writes=["aqT"])
                S.op("dve", L("tensor_copy", out=akT[i][:], in_=TB1[:, 12, :]), reads=["TB1b"], writes=[f"akT{i}"])
                S.op("dve", L("tensor_copy", out=ppT_[i][:], in_=TB1[:, 13:15, :]), reads=["TB1b"], writes=[f"ppT{i}"])
                yield None
                for h in range(8):
                    hp, j = h % 2, h // 2
                    bank = 3 + hp
                    S.op("pe", L("matmul", Fb[bank][:, j * 128:(j + 1) * 128],
                                 lhsT=rqkT[hp * 64:hp * 64 + 64, 4 + j, :], rhs=rqkT[hp * 64:hp * 64 + 64, j, :],
                                 start=True, stop=True),
                         reads=["rqkT"], writes=[f"F{bank}"], inc=(h >= 6))
                for hp in range(2):
                    S.op("dve", L("tensor_tensor", out=sd[:].rearrange("p (j t) q -> p t j q", t=2)[:, hp],
                                  in0=Fb[3 + hp][:].rearrange("p (j q) -> p j q", q=128),
                                  in1=dtm[:].rearrange("p (j t) q -> p t j q", t=2)[:, hp], op=ALU.mult),
                         reads=[f"F{3 + hp}"], writes=["sd"])
                for h in range(8):
                    hp, j = h % 2, h // 2
                    S.op("pe", L("matmul", Fb[0][:, h * 64:(h + 1) * 64], lhsT=sd[:, h, :], rhs=rv[:, h * 64:(h + 1) * 64],
                                                   start=True, stop=False), reads=["sd", "rv"], writes=["F0"], inc=False)
                    S.op("pe", L("matmul", Fb[0][:, h * 64:(h + 1) * 64], lhsT=rqTw[hp * 64:hp * 64 + 64, j, :],
                                                   rhs=stateb[hp * 64:hp * 64 + 64, j, :], start=False, stop=True),
                         reads=["rqTw", "stateb"], writes=["F0"], inc=(h == 7))
                for h in range(8):
                    hp, j = h % 2, h // 2
                    S.op("pe", L("matmul", Fb[1][hp * 64:hp * 64 + 64, j * 64:(j + 1) * 64], lhsT=kw[:, h, :],
                                                   rhs=rv[:, h * 64:(h + 1) * 64], start=True, stop=True),
                         reads=["kw", "rv"], writes=["F1"], inc=(h == 7))
                S.op("dve", L("tensor_tensor", out=sttmp[:], in0=state[:], in1=cdt[:], op=ALU.mult),
                     reads=["state"], writes=["sttmp"])
                S.op("dve", L("tensor_tensor", out=state[:], in0=sttmp[:], in1=Fb[1][:, 0:256].rearrange("p (j e) -> p j e", e=64),
                                                      op=ALU.add), reads=["sttmp", "F1"], writes=["state"])
                S.op("act", L("copy", out=stateb[:], in_=state[:]), reads=["state"], writes=["stateb"])
                pO3 = Fb[0][:].rearrange("p (h d) -> p h d", d=64)
                S.op("dve", L("tensor_reduce", out=st1[:], in_=pO3, axis=AX.X, op=ALU.add), reads=["F0"], writes=["st1"])
                S.op("act", L("activation", out=att_t[:].rearrange("p h d -> p (h d)"), in_=Fb[0][:], func=AF.Square), reads=["F0"], writes=["att_t"])
                S.op("dve", L("tensor_reduce", out=st2[:], in_=att_t[:], axis=AX.X, op=ALU.add),
                     reads=["att_t"], writes=["st2"])
                S.op("dve", L("tensor_scalar", out=stm[:], in0=st1[:], scalar1=1.0 / 64, scalar2=None, op0=ALU.mult),
                     reads=["st1"], writes=["stm"])
                S.op("dve", L("tensor_tensor", out=stv[:], in0=stm[:], in1=stm[:], op=ALU.mult), reads=["stm"], writes=["stv"])
                S.op("dve", L("scalar_tensor_tensor", out=stv[:], in0=st2[:], scalar=1.0 / 64, in1=stv[:], op0=ALU.mult, op1=ALU.subtract),
                     reads=["st2", "stv"], writes=["stv"])
                S.op("dve", L("tensor_scalar", out=stv[:], in0=stv[:], scalar1=1e-6, scalar2=None, op0=ALU.add),
                     reads=["stv"], writes=["stv"])
                S.op("act", L("activation", out=strs[:], in_=stv[:], func=AF.Ln), reads=["stv"], writes=["strs"])
                S.op("act", L("activation", out=strs[:], in_=strs[:], func=AF.Exp, scale=-0.5), reads=["strs"], writes=["strs"])
                S.op("dve", L("tensor_tensor", out=ropA[:], in0=pO3, in1=bc(stm[:, :], [128, 8, 64], 2), op=ALU.subtract),
                     reads=["F0", "stm"], writes=["ropA"])
                S.op("dve", L("tensor_tensor", out=gs[:], in0=sg[:], in1=gain[:], op=ALU.mult), reads=["sg"], writes=["gs"])
                S.op("dve", L("tensor_tensor", out=ropA[:], in0=ropA[:], in1=bc(strs[:, :], [128, 8, 64], 2), op=ALU.mult),
                     reads=["ropA", "strs"], writes=["ropA"])
                if os.environ.get("K_ALT") == "1":
                    S.op("dve", L("memset", mixcat_[i][:, 0:512], 0.0), reads=["ropA", "gs"], writes=[f"mixcat{i}"])
                elif os.environ.get("K_ALT") == "2":
                    S.op("dve", L("tensor_tensor", out=bigA[:, 0:512], in0=ropA[:].rearrange("p h d -> p (h d)"), in1=gs[:], op=ALU.mult),
                         reads=["ropA", "gs"], writes=[f"mixcat{i}"])
                else:
                    S.op("dve", L("tensor_tensor", out=mixcat_[i][:, 0:512], in0=ropA[:].rearrange("p h d -> p (h d)"), in1=gs[:], op=ALU.mult),
                         reads=["ropA", "gs"], writes=[f"mixcat{i}"])
                yield None
                blks = ([(1 - i, mprev, 0)] if n > 0 else []) + [(i, mcur, 1)]
                cnt_as = 0
                for (par, msk, bi) in blks:
                    for hk in range(2):
                        bank = 3 + (cnt_as % 2); cnt_as += 1
                        S.op("pe", L("matmul", Fb[bank][:], lhsT=akT[par][hk * 64:hk * 64 + 64, :],
                                                       rhs=aqT[hk * 64:hk * 64 + 64, :, :].rearrange("p g q -> p (g q)"),
                                                       start=True, stop=True),
                             reads=[f"akT{par}", "aqT"], writes=[f"F{bank}"])
                        S.op("act", L("activation", out=eT[bi][:, hk, :], in_=Fb[bank][:], func=AF.Exp, scale=0.125),
                             reads=[f"F{bank}"], writes=[f"eT{bi}"])
                    S.op("dve", L("tensor_tensor", out=eT[bi][:].rearrange("p k (g q) -> p (k g) q", q=128),
                                                           in0=eT[bi][:].rearrange("p k (g q) -> p (k g) q", q=128),
                                                           in1=bc(msk[:, :], [128, 8, 128], 1), op=ALU.mult),
                         reads=[f"eT{bi}"], writes=[f"eT{bi}"])
                pv_bank = {0: 2, 1: 1}
                for hk in range(2):
                    bank = pv_bank[hk]
                    for g in range(4):
                        for bidx, (par, msk, bi) in enumerate(blks):
                            S.op("pe", L("matmul", Fb[bank][:, g * 66:(g + 1) * 66], lhsT=eT[bi][:, hk, g * 128:(g + 1) * 128],
                                                           rhs=vaug[par][:, hk, :], start=(bidx == 0), stop=(bidx == len(blks) - 1)),
                                 reads=[f"eT{bi}", f"vaug{par}"], writes=[f"F{bank}"], inc=(g == 3 and bidx == len(blks) - 1))
                for hk in range(2):
                    bank = pv_bank[hk]
                    v4 = Fb[bank][:, 0:264].rearrange("p (g c) -> p g c", c=66)
                    S.op("dve", L("tensor_tensor", out=den[:, hk * 4:(hk + 1) * 4], in0=v4[:, :, 64], in1=esink[:, hk * 4:(hk + 1) * 4], op=ALU.add),
                         reads=[f"F{bank}", "esink"], writes=["den"])
                S.op("dve", L("reciprocal", out=rden[:], in_=den[:]), reads=["den"], writes=["rden"])
                for hk in range(2):
                    bank = pv_bank[hk]
                    v4 = Fb[bank][:, 0:264].rearrange("p (g c) -> p g c", c=66)
                    S.op("dve", L("tensor_tensor", out=att_t[:, hk * 4:(hk + 1) * 4, :], in0=v4[:, :, 0:64],
                                                          in1=bc(rden[:, hk * 4:(hk + 1) * 4], [128, 4, 64], 2), op=ALU.mult),
                         reads=[f"F{bank}", "rden"], writes=["att_t"])
                S.op("dve", L("tensor_tensor", out=mixcat_[i][:, 512:1024], in0=att_t[:].rearrange("p h d -> p (h d)"), in1=ascale[:], op=ALU.mult),
                     reads=["att_t"], writes=[f"mixcat{i}"])
                yield "HALF"
                for k in range(8):
                    S.op("pe", L("transpose", out=TB0[:, k, :], in_=mixcat_[i][:, k * 128:(k + 1) * 128], identity=identb[:]),
                         reads=[f"mixcat{i}"], writes=["TB0"], inc=(k == 7))
                S.op("act", L("copy", out=mixT[:], in_=TB0[:]), reads=["TB0"], writes=["mixT"])
                for hf in range(2):
                    for k in range(8):
                        S.op("pe", L("matmul", Fb[3 + hf][:], lhsT=mixT[:, k, :], rhs=woutb[:, k, hf * 512:(hf + 1) * 512],
                                                       start=(k == 0), stop=(k == 7)),
                             reads=["mixT"], writes=[f"F{3 + hf}"], inc=(k == 7))
                for hf in range(2):
                    S.op("dve", L("scalar_tensor_tensor", out=bigA[:, hf * 512:(hf + 1) * 512], in0=xf[i][:, hf * 512:(hf + 1) * 512],
                                                                 scalar=ALPHA, in1=Fb[3 + hf][:], op0=ALU.mult, op1=ALU.add),
                         reads=[f"xf{i}", f"F{3 + hf}"], writes=["bigA"])
                for hf in range(2):
                    S.op("dve", L("bn_stats", out=bnst[:, hf, :], in_=bigA[:, hf * 512:(hf + 1) * 512]), reads=["bigA"], writes=["bnst"])
                S.op("dve", L("bn_aggr", out=mv[:], in_=bnst[:].rearrange("p a b -> p (a b)")), reads=["bnst"], writes=["mv"])
                S.op("dve", L("tensor_scalar", out=rstd[:], in0=mv[:, 1:2], scalar1=1e-5, scalar2=None, op0=ALU.add),
                     reads=["mv"], writes=["rstd"])
                S.op("act", L("activation", out=rstd[:], in_=rstd[:], func=AF.Ln), reads=["rstd"], writes=["rstd"])
                S.op("act", L("activation", out=rstd[:], in_=rstd[:], func=AF.Exp, scale=-0.5), reads=["rstd"], writes=["rstd"])
                S.op("dve", L("scalar_tensor_tensor", out=nmr[:], in0=mv[:, 0:1], scalar=-1.0, in1=rstd[:], op0=ALU.mult, op1=ALU.mult),
                     reads=["mv", "rstd"], writes=["nmr"])
                S.op("act", L("activation", out=x1[:], in_=bigA[:], func=AF.Identity, bias=nmr[:, 0:1], scale=rstd[:, 0:1]),
                     reads=["bigA", "nmr", "rstd"], writes=["x1"])
                S.op("dve", L("tensor_tensor", out=x1[:], in0=x1[:], in1=ln1g[:], op=ALU.mult), reads=["x1"], writes=["x1"])
                S.op("dve", L("tensor_tensor", out=x1[:], in0=x1[:], in1=ln1b[:], op=ALU.add), reads=["x1"], writes=["x1"])
                yield None
                S.op("act", L("copy", out=xb[:], in_=x1[:]), reads=["x1"], writes=["xb"])
                S.dma("sp", x1d[n * 128:(n + 1) * 128, :], xb[:], d_st_x, reads=["xb"], writes=[f"x1d{n}"])
                x1T_ps = [Fb[0][:].rearrange("p (k t) -> p k t", t=128), Fb[1][:].rearrange("p (k t) -> p k t", t=128)]
                for k in range(8):
                    S.op("pe", L("transpose", out=x1T_ps[k // 4][:, k % 4, :], in_=x1[:, k * 128:(k + 1) * 128], identity=identf[:]),
                         reads=["x1"], writes=[f"F{k // 4}"], inc=(k % 4 == 3))
                for hf in range(2):
                    S.op("dve", L("tensor_copy", out=x1Tf[:, hf * 4:(hf + 1) * 4, :], in_=x1T_ps[hf]), reads=[f"F{hf}"], writes=["x1Tf"])
                    S.op("act", L("copy", out=x1Tb[:, hf * 4:(hf + 1) * 4, :], in_=x1T_ps[hf]), reads=[f"F{hf}"], writes=["x1Tb"])
                for k in range(8):
                    S.op("pe", L("matmul", Fb[2][:, 0:NE], lhsT=x1Tf[:, k, :], rhs=wr[:, k, :], start=(k == 0), stop=(k == 7)),
                         reads=["x1Tf"], writes=["F2"], inc=(k == 7))
                S.op("act", L("activation", out=scores[:], in_=Fb[2][:, 0:NE], func=AF.Sigmoid), reads=["F2"], writes=["scores"])
                hps = Fb[3][:].rearrange("p (m t) -> p m t", t=128)
                for m in range(4):
                    for k in range(8):
                        S.op("pe", L("matmul", hps[:, m, :], lhsT=wsb[:, k, m * 128:(m + 1) * 128], rhs=x1Tb[:, k, :],
                                                       start=(k == 0), stop=(k == 7)),
                             reads=["x1Tb"], writes=["F3"], inc=(m == 3 and k == 7))
                S.op("act", L("activation", out=sl[:], in_=hps[:, 0:2, :], func=AF.Silu), reads=["F3"], writes=["sl"])
                S.op("dve", L("tensor_tensor", out=hsT[:], in0=sl[:], in1=hps[:, 2:4, :], op=ALU.mult), reads=["sl", "F3"], writes=["hsT"])
                for hf in range(2):
                    for c in range(2):
                        S.op("pe", L("matmul", Fb[hf][:], lhsT=hsT[:, c, :], rhs=ws2b[:, c, hf * 512:(hf + 1) * 512],
                                                       start=(c == 0), stop=(c == 1)),
                             reads=["hsT"], writes=[f"F{hf}"], inc=(c == 1))
                for hf in range(2):
                    for k in range(8):
                        S.op("pe", L("matmul", Fb[3 + hf][:], lhsT=x1Tb[:, k, :], rhs=wgb[:, k, hf * 512:(hf + 1) * 512],
                                                       start=(k == 0), stop=False),
                             reads=["x1Tb"], writes=[f"F{3 + hf}"], inc=False)
                    S.op("pe", L("matmul", Fb[3 + hf][:], lhsT=onesb[0:1, :], rhs=bgb[0:1, hf * 512:(hf + 1) * 512], start=False, stop=True),
                         reads=["x1Tb"], writes=[f"F{3 + hf}"])
                for hf in range(2):
                    S.op("act", L("activation", out=bigA[:, hf * 512:(hf + 1) * 512], in_=Fb[3 + hf][:], func=AF.Sigmoid),
                         reads=[f"F{3 + hf}"], writes=["bigA"])
                for hf in range(2):
                    for c in range(2):
                        S.op("pe", L("matmul", Fb[3 + hf][:], lhsT=ppT_[i][:, c, :], rhs=wpb[:, c, hf * 512:(hf + 1) * 512],
                                                       start=(c == 0), stop=(c == 1)),
                             reads=[f"ppT{i}"], writes=[f"F{3 + hf}"], inc=(c == 1))
                for hf in range(2):
                    sl_ = slice(hf * 512, (hf + 1) * 512)
                    S.op("dve", L("tensor_tensor", out=bigC[:, sl_], in0=bigA[:, sl_], in1=Fb[3 + hf][:], op=ALU.mult),
                         reads=["bigA", f"F{3 + hf}"], writes=["bigC"])
                S.op("dve", L("scalar_tensor_tensor", out=bigC[:], in0=x1[:], scalar=ALPHA, in1=bigC[:], op0=ALU.mult, op1=ALU.add),
                     reads=["x1", "bigC"], writes=["bigC"])
                for hf in range(2):
                    sl_ = slice(hf * 512, (hf + 1) * 512)
                    S.op("dve", L("tensor_tensor", out=bigC[:, sl_], in0=bigC[:, sl_], in1=Fb[hf][:], op=ALU.add),
                         reads=["bigC", f"F{hf}"], writes=["bigC"])
                S.dma("sp", based[n * 128:(n + 1) * 128, :], bigC[:], d_st_b, reads=["bigC"], writes=[f"based{n}"])
                yield None
                V = lambda fn, r, w: S.op("dve", fn, reads=r, writes=w)
                V(L("tensor_tensor", out=biased[:], in0=scores[:], in1=rbias[:], op=ALU.add), ["scores"], ["biased"])
                b3 = biased[:].rearrange("p (g k) -> p g k", k=8)
                V(L("tensor_reduce", out=r_m1[:], in_=b3, axis=AX.X, op=ALU.max), ["biased"], ["r_m1"])
                V(L("tensor_tensor", out=r_eq[:].rearrange("p (g k) -> p g k", k=8), in0=b3, in1=bc(r_m1[:, :], [128, 8, 8], 2), op=ALU.is_equal),
                  ["biased", "r_m1"], ["r_eq"])
                V(L("scalar_tensor_tensor", out=r_b2[:], in0=r_eq[:], scalar=-1e30, in1=biased[:], op0=ALU.mult, op1=ALU.add),
                  ["r_eq", "biased"], ["r_b2"])
                V(L("tensor_reduce", out=r_m2[:], in_=r_b2[:].rearrange("p (g k) -> p g k", k=8), axis=AX.X, op=ALU.max), ["r_b2"], ["r_m2"])
                V(L("tensor_tensor", out=r_gs[:], in0=r_m1[:], in1=r_m2[:], op=ALU.add), ["r_m1", "r_m2"], ["r_gs"])
                V(L("max", out=r_gtop[:], in_=r_gs[:]), ["r_gs"], ["r_gtop"])
                V(L("tensor_scalar", out=r_gm[:], in0=r_gs[:], scalar1=r_gtop[:, 3:4], scalar2=None, op0=ALU.is_ge), ["r_gs", "r_gtop"], ["r_gm"])
                V(L("tensor_scalar", out=r_neg[:], in0=r_gm[:], scalar1=1e30, scalar2=-1e30, op0=ALU.mult, op1=ALU.add), ["r_gm"], ["r_neg"])
                m3 = r_msk[:].rearrange("p (g k) -> p g k", k=8)
                V(L("tensor_tensor", out=m3, in0=b3, in1=bc(r_gm[:, :], [128, 8, 8], 2), op=ALU.mult), ["biased", "r_gm"], ["r_msk"])
                V(L("tensor_tensor", out=m3, in0=m3, in1=bc(r_neg[:, :], [128, 8, 8], 2), op=ALU.add), ["r_msk", "r_neg"], ["r_msk"])
                V(L("max", out=r_top[:], in_=r_msk[:]), ["r_msk"], ["r_top"])
                V(L("tensor_scalar", out=r_M[:], in0=r_msk[:], scalar1=r_top[:, 7:8], scalar2=None, op0=ALU.is_ge), ["r_msk", "r_top"], ["r_M"])
                V(L("tensor_tensor", out=r_w[:], in0=scores[:], in1=r_M[:], op=ALU.mult), ["scores", "r_M"], ["r_w"])
                V(L("tensor_reduce", out=r_ws[:], in_=r_w[:], axis=AX.X, op=ALU.add), ["r_w"], ["r_ws"])
                V(L("reciprocal", out=r_rs[:], in_=r_ws[:]), ["r_ws"], ["r_rs"])
                V(L("tensor_scalar", out=cm[:], in0=r_w[:], scalar1=r_rs[:, 0:1], scalar2=2.5, op0=ALU.mult, op1=ALU.mult), ["r_w", "r_rs"], ["cm"])
                S.dma("sp", cmatd[n * 128:(n + 1) * 128, :], cm[:], d_st_c, reads=["cm"], writes=[f"cmatd{n}"])
                S.op("act", L("copy", out=r_Mb[:], in_=r_M[:]), reads=["r_M"], writes=["r_Mb"])
                S.op("pe", L("matmul", Fb[2][:, 64:128], lhsT=utri[:], rhs=r_Mb[:], start=True, stop=True), reads=["r_Mb"], writes=["F2"], inc=False)
                S.op("pe", L("matmul", Fb[2][:, 128:192], lhsT=onesb[:], rhs=r_Mb[:], start=True, stop=True), reads=["r_Mb"], writes=["F2"])
                V(L("tensor_tensor", out=r_pos[:], in0=Fb[2][:, 64:128], in1=r_run[:], op=ALU.add), ["F2", "r_run"], ["r_pos"])
                V(L("tensor_tensor", out=r_run[:], in0=Fb[2][:, 128:192], in1=r_run[:], op=ALU.add), ["F2", "r_run"], ["r_run"])
                V(L("tensor_scalar", out=r_val[:], in0=r_pos[:], scalar1=float(CAP), scalar2=None, op0=ALU.is_lt), ["r_pos"], ["r_val"])
                V(L("tensor_tensor", out=r_key[:], in0=r_pos[:], in1=eoff[:], op=ALU.add), ["r_pos"], ["r_key"])
                V(L("tensor_tensor", out=r_key[:], in0=r_key[:], in1=r_M[:], op=ALU.mult), ["r_key", "r_M"], ["r_key"])
                V(L("tensor_tensor", out=r_key[:], in0=r_key[:], in1=r_val[:], op=ALU.mult), ["r_key", "r_val"], ["r_key"])
                V(L("max", out=r_k8[:], in_=r_key[:]), ["r_key"], ["r_k8"])
                V(L("tensor_scalar", out=r_z[:], in0=r_k8[:], scalar1=0.5, scalar2=float(DUMMY_SLOT + 1), op0=ALU.is_lt, op1=ALU.mult), ["r_k8"], ["r_z"])
                V(L("scalar_tensor_tensor", out=r_f8[:], in0=r_k8[:], scalar=-1.0, in1=r_z[:], op0=ALU.add, op1=ALU.add), ["r_k8", "r_z"], ["r_f8"])
                V(L("tensor_copy", out=slots[:, n, :], in_=r_f8[:]), ["r_f8"], [f"slots{n}"])
                for k in range(8):
                    S.idma(tl, bass.IndirectOffsetOnAxis(ap=slots[:, n, k:k + 1], axis=0), tokc[:, n:n + 1], None, d_sc,
                           reads=[f"slots{n}"], writes=[f"tlw{n}_{k}"])
            def run_first(g):
                for v in g:
                    if v == "HALF":
                        return
            def step(g):
                try:
                    next(g)
                    return True
                except StopIteration:
                    return False
            prev = None
            for n in range(nrun):
                g = chunk(n)
                if prev is None:
                    run_first(g)
                else:
                    first_done = False; second_done = False
                    while not (first_done and second_done):
                        if not first_done:
                            try:
                                v = next(g)
                                if v == "HALF":
                                    first_done = True
                            except StopIteration:
                                first_done = True
                        if not second_done:
                            second_done = not step(prev)
                prev = g
            while step(prev):
                pass
            S.barrier([d_init, d_st_x, d_st_b, d_st_c, d_sc] + d_x + d_p + d_r)
            S.flush()
            if upto == 1:
                return nc

        with ExitStack() as st:
            sb = lambda n, s, d=F32: st.enter_context(nc.sbuf_tensor(n, list(s), d))
            ps = lambda n, s, d=F32: st.enter_context(nc.psum_tensor(n, list(s), d))
            NW = 3
            w13b = [sb(f"w13b{i}", [128, 8, 512], BF16) for i in range(NW)]
            w2b = [sb(f"w2b{i}", [128, 2, D], BF16) for i in range(NW)]
            idx = [sb(f"idx{i}", [128, NJ], I32) for i in range(2)]
            cg = [sb(f"cg{i}", [128, NJ, NE]) for i in range(2)]
            xg = [sb(f"xg{i}", [128, NJ, D], BF16) for i in range(2)]
            xgT = [sb(f"xgT{i}", [128, 8, CAP], BF16) for i in range(2)]
            slh = [sb(f"slh{i}", [128, 512]) for i in range(2)]
            hT = sb("hT", [128, 2, CAP], BF16)
            ys = [sb(f"ys{i}", [128, NJ, D]) for i in range(2)]
            TP = [ps(f"TP{i}", [128, 8, 128], BF16) for i in range(2)]
            HB = [ps(f"HB{i}", [128, 512]) for i in range(4)]
            YB = [ps(f"YB{i}", [128, 512]) for i in range(2)]
            d_wt = [S.dsem(f"wt{i}") for i in range(NW)]
            d_g = [S.dsem(f"g{i}") for i in range(2)]
            d_i = [S.dsem(f"i{i}") for i in range(2)]
            d_y = [[S.dsem(f"y{i}_{j_}") for j_ in range(NJ)] for i in range(2)]

            def load_w(e_):
                b = e_ % NW
                S.dma("pool", w13b[b][:, :, 0:256], w1[e_].rearrange("(k p) n -> p k n", p=128), d_wt[b], writes=[f"wts{b}"])
                S.dma("pool", w13b[b][:, :, 256:512], w3[e_].rearrange("(k p) n -> p k n", p=128), d_wt[b], writes=[f"wts{b}"])
                S.dma("pool", w2b[b][:], w2[e_].rearrange("(k p) n -> p k n", p=128), d_wt[b], writes=[f"wts{b}"])

            def load_g(e_):
                b = e_ % 2
                S.dma("sp", idx[b][:], tl[e_ * CAP:(e_ + 1) * CAP, :].rearrange("(p j) o -> p (j o)", j=NJ), d_i[b],
                      writes=[f"idx{b}"])
                for j in range(NJ):
                    S.idma(xg[b][:, j, :], None, x1d, bass.IndirectOffsetOnAxis(ap=idx[b][:, j:j + 1], axis=0), d_g[b],
                           reads=[f"idx{b}"], writes=[f"gath{b}"])
                    S.idma(cg[b][:, j, :], None, cmatd, bass.IndirectOffsetOnAxis(ap=idx[b][:, j:j + 1], axis=0), d_g[b],
                           reads=[f"idx{b}"], writes=[f"gath{b}"])
                S.readers[f"idx{b}"] = [(d_g[b], S.cnt[d_g[b]])]

            tsl = [(0, 512), (512, CAP - 512)] if CAP > 512 else [(0, CAP)]
            load_w(0); load_g(0); load_w(1)
            for e_ in range(NE):
                b = e_ % 2; wb_ = e_ % NW
                if e_ + 1 < NE:
                    load_g(e_ + 1)
                if e_ + 2 < NE:
                    load_w(e_ + 2)
                for j in range(NJ):
                    tp = j % 2
                    for k in range(8):
                        S.op("pe", L("transpose", out=TP[tp][:, k, :], in_=xg[b][:, j, k * 128:(k + 1) * 128], identity=identb[:]),
                             reads=[f"gath{b}"], writes=[f"TP{tp}"], inc=(k == 7))
                    eng = "act" if j % 2 == 0 else "dve"
                    if eng == "act":
                        S.op("act", L("copy", out=xgT[b][:, :, j * 128:(j + 1) * 128], in_=TP[tp][:]), reads=[f"TP{tp}"], writes=[f"xgT{b}"])
                    else:
                        S.op("dve", L("tensor_copy", out=xgT[b][:, :, j * 128:(j + 1) * 128], in_=TP[tp][:]), reads=[f"TP{tp}"], writes=[f"xgT{b}"])
                for ti, (t0, tn) in enumerate(tsl):
                    for m in range(4):
                        for k in range(8):
                            S.op("pe", L("matmul", HB[m][:, 0:tn], lhsT=w13b[wb_][:, k, m * 128:(m + 1) * 128], rhs=xgT[b][:, k, t0:t0 + tn],
                                                           start=(k == 0), stop=(k == 7)),
                                 reads=[f"wts{wb_}", f"xgT{b}"], writes=[f"HB{m}"], inc=(k == 7))
                    for m in range(2):
                        S.op("act", L("activation", out=slh[m][:, 0:tn], in_=HB[m][:, 0:tn], func=AF.Silu), reads=[f"HB{m}"], writes=[f"slh{m}"])
                        S.op("dve", L("tensor_tensor", out=hT[:, m, t0:t0 + tn], in0=slh[m][:, 0:tn], in1=HB[2 + m][:, 0:tn], op=ALU.mult),
                             reads=[f"slh{m}", f"HB{2 + m}"], writes=["hT"])
                for j in range(NJ):
                    for hf in range(2):
                        for c in range(2):
                            S.op("pe", L("matmul", YB[hf][:], lhsT=hT[:, c, j * 128:(j + 1) * 128], rhs=w2b[wb_][:, c, hf * 512:(hf + 1) * 512],
                                                           start=(c == 0), stop=(c == 1)),
                                 reads=["hT", f"wts{wb_}"], writes=[f"YB{hf}"], inc=(c == 1))
                    S.op("act", L("activation", out=ys[b][:, j, 0:512], in_=YB[0][:], func=AF.Copy, scale=cg[b][:, j, e_:e_ + 1]),
                         reads=["YB0", f"gath{b}"], writes=[f"ysA{b}_{j}"])
                    S.op("dve", L("tensor_scalar", out=ys[b][:, j, 512:1024], in0=YB[1][:], scalar1=cg[b][:, j, e_:e_ + 1], scalar2=None, op0=ALU.mult),
                         reads=["YB1", f"gath{b}"], writes=[f"ysD{b}_{j}"])
                    S.dma("sp", Yd[e_ * CAP:(e_ + 1) * CAP, :].rearrange("(p j) d -> p j d", j=NJ)[:, j, :], ys[b][:, j, :], d_y[b][j],
                          reads=[f"ysA{b}_{j}", f"ysD{b}_{j}"], writes=[f"Yd{e_}_{j}"])
            S.barrier([d for row in d_y for d in row])
            S.flush()
            if upto == 2:
                return nc

        with ExitStack() as st:
            sb = lambda n, s, d=F32: st.enter_context(nc.sbuf_tensor(n, list(s), d))
            bt = [sb(f"bt{i}", [128, D]) for i in range(2)]
            yk = [sb(f"yk{i}", [128, 8, D]) for i in range(2)]
            acc = [sb(f"acc{i}", [128, D]) for i in range(2)]
            accp = [sb(f"accp{i}", [128, D]) for i in range(2)]
            bn2 = sb("bn2", [128, 2, 6]); mv2 = sb("mv2", [128, 2]); rs2 = sb("rs2", [128, 1]); nh2 = sb("nh2", [128, 1]); nm2 = sb("nm2", [128, 1])
            d_b = [S.dsem(f"b{i}") for i in range(2)]
            d_k = [S.dsem(f"k{i}") for i in range(2)]
            d_o = [S.dsem(f"o{i}") for i in range(2)]
            S.op("dve", L("memset", nh2[:], -0.5), writes=["nh2"])

            def load3(n):
                i = n % 2
                S.dma("sp", bt[i][:], based[n * 128:(n + 1) * 128, :], d_b[i], writes=[f"bt{i}"])
                for k in range(8):
                    S.idma(yk[i][:, k, :], None, Yd, bass.IndirectOffsetOnAxis(ap=slots[:, n, k:k + 1], axis=0), d_k[i],
                           writes=[f"yk{i}"])

            load3(0)
            for n in range(NCH):
                i = n % 2
                if n + 1 < NCH:
                    load3(n + 1)
                S.op("dve", L("tensor_tensor", out=acc[i][:], in0=yk[i][:, 0, :], in1=yk[i][:, 1, :], op=ALU.add), reads=[f"yk{i}"], writes=[f"acc{i}"])
                S.op("dve", L("tensor_tensor", out=accp[i][:], in0=yk[i][:, 4, :], in1=yk[i][:, 5, :], op=ALU.add), reads=[f"yk{i}"], writes=[f"accp{i}"])
                for k in (2, 3):
                    S.op("dve", L("tensor_tensor", out=acc[i][:], in0=acc[i][:], in1=yk[i][:, k, :], op=ALU.add), reads=[f"yk{i}", f"acc{i}"], writes=[f"acc{i}"])
                for k in (6, 7):
                    S.op("dve", L("tensor_tensor", out=accp[i][:], in0=accp[i][:], in1=yk[i][:, k, :], op=ALU.add), reads=[f"yk{i}", f"accp{i}"], writes=[f"accp{i}"])
                S.op("dve", L("tensor_tensor", out=accp[i][:], in0=accp[i][:], in1=bt[i][:], op=ALU.add), reads=[f"bt{i}", f"accp{i}"], writes=[f"accp{i}"])
                S.op("dve", L("tensor_tensor", out=acc[i][:], in0=acc[i][:], in1=accp[i][:], op=ALU.add), reads=[f"accp{i}", f"acc{i}"], writes=[f"acc{i}"])
                for hf in range(2):
                    S.op("dve", L("bn_stats", out=bn2[:, hf, :], in_=acc[i][:, hf * 512:(hf + 1) * 512]), reads=[f"acc{i}"], writes=["bn2"])
                S.op("dve", L("bn_aggr", out=mv2[:], in_=bn2[:].rearrange("p a b -> p (a b)")), reads=["bn2"], writes=["mv2"])
                S.op("dve", L("tensor_scalar", out=rs2[:], in0=mv2[:, 1:2], scalar1=1e-5, scalar2=None, op0=ALU.add), reads=["mv2"], writes=["rs2"])
                S.op("act", L("activation", out=rs2[:], in_=rs2[:], func=AF.Ln), reads=["rs2"], writes=["rs2"])
                S.op("act", L("activation", out=rs2[:], in_=rs2[:], func=AF.Exp, scale=-0.5), reads=["rs2"], writes=["rs2"])
                S.op("dve", L("scalar_tensor_tensor", out=nm2[:], in0=mv2[:, 0:1], scalar=-1.0, in1=rs2[:], op0=ALU.mult, op1=ALU.mult),
                     reads=["mv2", "rs2"], writes=["nm2"])
                S.op("act", L("activation", out=acc[i][:], in_=acc[i][:], func=AF.Identity, bias=nm2[:, 0:1], scale=rs2[:, 0:1]),
                     reads=[f"acc{i}", "nm2", "rs2"], writes=[f"acc{i}"])
                S.op("dve", L("tensor_tensor", out=acc[i][:], in0=acc[i][:], in1=ln2g[:], op=ALU.mult), reads=[f"acc{i}", "ln2g"], writes=[f"acc{i}"])
                S.op("dve", L("tensor_tensor", out=acc[i][:], in0=acc[i][:], in1=ln2b[:], op=ALU.add), reads=[f"acc{i}", "ln2b"], writes=[f"acc{i}"])
                S.dma("sp", out[n * 128:(n + 1) * 128, :], acc[i][:], d_o[i], reads=[f"acc{i}"], writes=[f"out{n}"])
            S.wait_all("sp", [f"out{n}" for n in range(NCH)])
            S.wait_all("pool", [f"out{n}" for n in range(NCH)])
            S.flush()
    return nc


def _consts():
    h = np.arange(8, dtype=np.float64)
    gam = 1.0 - 2.0 ** (-5.0 - h)
    lg = np.log(gam)
    idx = np.arange(128, dtype=np.float64)
    pos = np.arange(S_LEN, dtype=np.float32)
    def tabs(half, theta):
        inv = (np.float32(theta) ** (-(np.arange(half, dtype=np.float32) / np.float32(half)))).astype(np.float32)
        ang = (pos[:, None] * inv[None, :]).astype(np.float32)
        c, s = np.cos(ang).astype(np.float32), np.sin(ang).astype(np.float32)
        return np.concatenate([c, c], 1), np.concatenate([-s, s], 1)
    ccr, ssr = tabs(32, 10000.0)
    cca, ssa = tabs(8, 500000.0)
    rope = np.concatenate([ccr, ssr, cca, ssa], 1).reshape(NCH, 128, 160).astype(np.float32)
    diff = idx[None, :] - idx[:, None]
    dt = np.where(diff[:, None, :] >= 0, np.exp(np.maximum(diff, 0)[:, None, :] * lg[None, :, None]), 0.0) / 8.0
    qw = np.zeros((128, 4, 128)); cd = np.zeros((128, 4, 64))
    for hp in range(2):
        for j in range(4):
            hh = 2 * j + hp
            qw[hp * 64:(hp + 1) * 64, j, :] = np.exp((idx + 1) * lg[hh])[None, :]
            cd[hp * 64:(hp + 1) * 64, j, :] = np.exp(128 * lg[hh])
    kwt = np.exp((127 - idx)[:, None] * lg[None, :]) / 8.0
    kk = idx[:, None]; qq = idx[None, :]
    mcur = (kk <= qq).astype(np.float32)
    mprev = (kk > qq).astype(np.float32)
    utri = (kk < qq).astype(np.float32)
    eoff = np.tile((np.arange(NE, dtype=np.float32) * CAP + 1.0)[None, :], (128, 1))
    tok = (np.arange(NCH)[None, :] * 128 + np.arange(128)[:, None]).astype(np.int32)
    f = lambda a: np.ascontiguousarray(a, dtype=np.float32)
    return {"c_ident": f(np.eye(128)), "c_rope": f(rope), "c_dt": f(dt.reshape(128, 1024)), "c_qw": f(qw.reshape(128, 512)),
            "c_kw": f(kwt), "c_cd": f(cd.reshape(128, 256)), "c_mcur": f(mcur), "c_mprev": f(mprev), "c_utri": f(utri),
            "c_ones": f(np.ones((128, 128))), "c_eoff": f(eoff), "c_tok": np.ascontiguousarray(tok)}


def _rep(v, n=128):
    return np.ascontiguousarray(np.broadcast_to(np.asarray(v, dtype=np.float32).reshape(1, -1), (n, v.size)))


def make_in_maps(x, p, w_in, ret_gn_gain, attn_scale, sinks, w_out, ln1_g, ln1_b, w_router, router_bias,
                 w1, w3, w2, ws1, ws3, ws2, w_ple_gate, b_ple_gate, w_ple_proj, ln2_g, ln2_b):
    c = _consts()
    shared = {
        "w_in": np.ascontiguousarray(w_in[0]), "w_out": np.ascontiguousarray(w_out[0]), "w_router": np.ascontiguousarray(w_router[0]),
        "w1": np.ascontiguousarray(w1[0]), "w3": np.ascontiguousarray(w3[0]), "w2": np.ascontiguousarray(w2[0]),
        "ws1": np.ascontiguousarray(ws1[0]), "ws3": np.ascontiguousarray(ws3[0]), "ws2": np.ascontiguousarray(ws2[0]),
        "w_ple_gate": np.ascontiguousarray(w_ple_gate[0]), "w_ple_proj": np.ascontiguousarray(w_ple_proj[0]),
        "gain_r": _rep(ret_gn_gain[0]), "ascale_r": _rep(attn_scale[0]), "sinks_r": _rep(sinks[0]),
        "ln1g_r": _rep(ln1_g[0]), "ln1b_r": _rep(ln1_b[0]), "bgate_r": _rep(b_ple_gate[0]),
        "ln2g_r": _rep(ln2_g[0]), "ln2b_r": _rep(ln2_b[0]), "rbias_r": _rep(router_bias[0]),
    }
    shared.update(c)
    maps = []
    for b in range(8):
        m = dict(shared)
        m["x"] = np.ascontiguousarray(x[b]); m["p"] = np.ascontiguousarray(p[0, b])
        maps.append(m)
    return maps


def kernel(**inputs):
    inputs = {k: np.asarray(v) for k, v in inputs.items()}
    nc = build()
    maps = make_in_maps(**inputs)
    res = run_bass_kernel_spmd(nc, maps, core_ids=list(range(8)))
    return np.stack([np.asarray(r["out"], dtype=np.float32) for r in res.results], axis=0)
```
